# Optimizing a Trainium2 kernel written in Bass

```python
import math
import jax, jax.numpy as jnp
from jax import lax
import numpy as np

D_MODEL = 1024
BATCH = 4
SEQ = 8192
DEPTH = 1

MOBA_HEADS = 8
MOBA_HEAD_DIM = 64
MOBA_WIDTH = MOBA_HEADS * MOBA_HEAD_DIM
MOBA_BLOCK = 256
MOBA_TOPK = 3
MOBA_QBLOCK = 64
REL_BUCKETS = 32
REL_MAX_DIST = 128
GLA_HEADS = 4
GLA_KEY_DIM = D_MODEL // 2
GLA_VALUE_DIM = D_MODEL
GLA_GATE_RANK = 16
GLA_GATE_NORMALIZER = 16.0
GLA_CHUNK = 64
N_EXPERTS = 32
TOP_K = 4
D_FF = D_MODEL
SWIGLU_ALPHA = 1.702
SWIGLU_LIMIT = 7.0
MOE_BLOCK = 256
N_BRANCHES = 2
EPS = 1e-6
IN_SIZES = (MOBA_WIDTH, MOBA_WIDTH, MOBA_WIDTH,
            GLA_KEY_DIM, GLA_KEY_DIM, GLA_VALUE_DIM, GLA_GATE_RANK, GLA_VALUE_DIM,
            N_BRANCHES * D_MODEL)
D_IN = sum(IN_SIZES)

kernel_name = 'hybrid_moba_gla_moe_block'


def rmsnorm(x, w):
    xf = x.astype(jnp.float32)
    xf = xf * lax.rsqrt(jnp.mean(xf * xf, axis=-1, keepdims=True) + EPS)
    return xf.astype(x.dtype) * w


def modulate(h, shift, scale):
    return h * (1 + scale[:, None, :]) + shift[:, None, :]


def t5_bucket(q_pos, k_pos):
    n = jnp.maximum(q_pos - k_pos, 0)
    max_exact = REL_BUCKETS // 2
    nf = jnp.maximum(n, max_exact).astype(jnp.float32)
    large = max_exact + (jnp.log(nf / max_exact) / math.log(REL_MAX_DIST / max_exact)
                         * (REL_BUCKETS - max_exact)).astype(jnp.int32)
    large = jnp.minimum(large, REL_BUCKETS - 1)
    return jnp.where(n < max_exact, n, large)


def moba_attention(q, k, v, rel_bias):
    B, H, S, hd = q.shape
    n_blk = -(-S // MOBA_BLOCK)
    s_pad = n_blk * MOBA_BLOCK
    pad = ((0, 0), (0, 0), (0, s_pad - S), (0, 0))
    q = jnp.pad(q * hd ** -0.5, pad)
    k = jnp.pad(k, pad)
    v = jnp.pad(v, pad)
    k_blocks = k.reshape(B, H, n_blk, MOBA_BLOCK, hd)
    v_blocks = v.reshape(B, H, n_blk, MOBA_BLOCK, hd)
    k_mean = jnp.mean(k_blocks, axis=3)
    q_blk = jnp.arange(s_pad) // MOBA_BLOCK
    gate = jnp.einsum('bhsd,bhnd->bhsn', q, k_mean).astype(jnp.float32)
    past = jnp.arange(n_blk)[None, :] < q_blk[:, None]
    gate = jnp.where(past, gate, -jnp.inf)
    n_sel = min(MOBA_TOPK, n_blk)
    _, sel = lax.top_k(gate, n_sel)
    b_idx = jnp.arange(B)[:, None, None, None]
    h_idx = jnp.arange(H)[None, :, None, None]
    bias_f = rel_bias.astype(jnp.float32)
    bias_hb = bias_f.T
    blk_off = jnp.arange(MOBA_BLOCK)

    def query_block(t0):
        q_c = lax.dynamic_slice_in_dim(q, t0, MOBA_QBLOCK, axis=2)
        sel_c = lax.dynamic_slice_in_dim(sel, t0, MOBA_QBLOCK, axis=2)
        q_pos = t0 + jnp.arange(MOBA_QBLOCK)
        own0 = (t0 // MOBA_BLOCK) * MOBA_BLOCK
        k_sel = k_blocks[b_idx, h_idx, sel_c]
        v_sel = v_blocks[b_idx, h_idx, sel_c]
        k_pos_sel = sel_c[..., None] * MOBA_BLOCK + blk_off
        bias_sel = bias_hb[h_idx[..., None], t5_bucket(q_pos[:, None, None], k_pos_sel)]
        valid_sel = jnp.arange(n_sel)[None, :] < (q_pos // MOBA_BLOCK)[:, None]
        logit_sel = jnp.einsum('bhqd,bhqnkd->bhqnk', q_c, k_sel).astype(jnp.float32) + bias_sel
        logit_sel = jnp.where(valid_sel[:, :, None], logit_sel, -jnp.inf)
        logit_sel = logit_sel.reshape(B, H, MOBA_QBLOCK, n_sel * MOBA_BLOCK)
        k_own = lax.dynamic_slice_in_dim(k, own0, MOBA_BLOCK, axis=2)
        v_own = lax.dynamic_slice_in_dim(v, own0, MOBA_BLOCK, axis=2)
        k_pos_own = own0 + blk_off
        bias_own = jnp.moveaxis(bias_f[t5_bucket(q_pos[:, None], k_pos_own[None, :])], -1, 0)
        logit_own = jnp.einsum('bhqd,bhkd->bhqk', q_c, k_own).astype(jnp.float32) + bias_own
        logit_own = jnp.where(k_pos_own[None, :] <= q_pos[:, None], logit_own, -jnp.inf)
        p = jax.nn.softmax(jnp.concatenate([logit_sel, logit_own], axis=-1), axis=-1).astype(v.dtype)
        p_sel = p[..., :n_sel * MOBA_BLOCK].reshape(B, H, MOBA_QBLOCK, n_sel, MOBA_BLOCK)
        p_own = p[..., n_sel * MOBA_BLOCK:]
        return (jnp.einsum('bhqnk,bhqnkd->bhqd', p_sel, v_sel)
                + jnp.einsum('bhqk,bhkd->bhqd', p_own, v_own))

    out = lax.map(query_block, jnp.arange(s_pad // MOBA_QBLOCK) * MOBA_QBLOCK)
    return jnp.moveaxis(out, 0, 2).reshape(B, H, s_pad, hd)[:, :, :S]


def gla_chunked(q, k, v, log_a):
    B, S, H, dk = q.shape
    dv = v.shape[-1]
    nc = S // GLA_CHUNK

    def chunks(t):
        return jnp.moveaxis(t.reshape(B, nc, GLA_CHUNK, H, t.shape[-1]), 3, 1)

    dt = v.dtype
    q = chunks(q) * dk ** -0.5
    k = chunks(k)
    v = chunks(v)
    b = jnp.cumsum(chunks(log_a), axis=3)
    b_last = b[:, :, :, -1:, :]
    q_g = q * jnp.exp(b).astype(dt)
    k_g = k * jnp.exp(-b).astype(dt)
    k_end = k * jnp.exp(b_last - b).astype(dt)
    causal = jnp.tril(jnp.ones((GLA_CHUNK, GLA_CHUNK), dtype=bool))
    att = jnp.where(causal, jnp.einsum('bhncd,bhnsd->bhncs', q_g, k_g), 0)
    o_intra = jnp.einsum('bhncs,bhnse->bhnce', att, v)
    decay = jnp.exp(b_last[:, :, :, 0, :]).astype(dt)

    def step(state, xs):
        q_n, k_n, v_n, d_n = xs
        o_n = jnp.einsum('bhcd,bhde->bhce', q_n, state)
        state = d_n[..., None] * state + jnp.einsum('bhcd,bhce->bhde', k_n, v_n)
        return state, o_n

    xs = (jnp.moveaxis(q_g, 2, 0), jnp.moveaxis(k_end, 2, 0), jnp.moveaxis(v, 2, 0), jnp.moveaxis(decay, 2, 0))
    _, o_inter = lax.scan(step, jnp.zeros((B, H, dk, dv), dt), xs)
    o = o_intra + jnp.moveaxis(o_inter, 0, 2)
    return jnp.moveaxis(o, 1, 3).reshape(B, S, H, dv)


def moe_ffn(h, w_router, b_router, w_gate, b_gate, w_up, b_up, w_down, b_down):
    B, S, D = h.shape
    T = B * S
    A = T * TOP_K
    hf = h.reshape(T, D)
    logits = (hf @ w_router + b_router).astype(jnp.float32)
    top_vals, top_idx = lax.top_k(logits, TOP_K)
    weights = jax.nn.softmax(top_vals, axis=-1).astype(h.dtype)
    expert_ids = top_idx.reshape(A)
    token_of = jnp.arange(A) // TOP_K
    order = jnp.argsort(expert_ids)
    sorted_e = expert_ids[order]
    counts = jnp.bincount(expert_ids, length=N_EXPERTS)
    padded = (counts + MOE_BLOCK - 1) // MOE_BLOCK * MOE_BLOCK
    pcum = jnp.cumsum(padded)
    start = jnp.cumsum(counts) - counts
    pstart = pcum - padded
    dest_sorted = pstart[sorted_e] + (jnp.arange(A) - start[sorted_e])
    dest = jnp.zeros((A,), jnp.int32).at[order].set(dest_sorted.astype(jnp.int32))
    n_blocks = (A + MOE_BLOCK - 1) // MOE_BLOCK + N_EXPERTS
    n_pad = n_blocks * MOE_BLOCK
    slot_token = jnp.zeros((n_pad,), jnp.int32).at[dest].set(token_of)
    x_pad = hf[slot_token].reshape(n_blocks, MOE_BLOCK, D)
    block_expert = jnp.minimum(jnp.searchsorted(pcum, jnp.arange(n_blocks) * MOE_BLOCK, side='right'),
                               N_EXPERTS - 1)

    def expert_block(args):
        xb, e = args
        g = jnp.minimum(xb @ w_gate[e] + b_gate[e], SWIGLU_LIMIT)
        u = jnp.clip(xb @ w_up[e] + b_up[e], -SWIGLU_LIMIT, SWIGLU_LIMIT)
        act = g * jax.nn.sigmoid(SWIGLU_ALPHA * g) * (u + 1)
        return act @ w_down[e] + b_down[e]

    y_pad = lax.map(expert_block, (x_pad, block_expert)).reshape(n_pad, D)
    y = y_pad[dest].reshape(T, TOP_K, D)
    return jnp.einsum('tkd,tk->td', y, weights).reshape(B, S, D)


def setup_inputs(seed: int = 0) -> dict:
    key = jax.random.key(seed)
    it = iter(jax.random.split(key, 24))
    L, D, E, F = DEPTH, D_MODEL, N_EXPERTS, D_FF

    def nrm(shape, fan_in, s=1.0):
        return jax.random.normal(next(it), shape, jnp.float32) * (s * fan_in ** -0.5)

    def gain(shape):
        return 1.0 + 0.05 * jax.random.normal(next(it), shape, jnp.float32)

    def small(shape, s=0.01):
        return s * jax.random.normal(next(it), shape, jnp.float32)

    return {
        'x': jax.random.normal(next(it), (BATCH, SEQ, D), jnp.float32),
        'c': jax.random.normal(next(it), (BATCH, D), jnp.float32),
        'rel_bias': small((REL_BUCKETS, MOBA_HEADS), 0.5),
        'w_ada': nrm((L, D, 6 * D), D, 0.5),
        'b_ada': small((L, 6 * D)),
        'norm_mix': gain((L, D)),
        'w_in': nrm((L, D, D_IN), D),
        'w_gk_up': nrm((L, GLA_GATE_RANK, GLA_KEY_DIM), GLA_GATE_RANK),
        'b_gk': small((L, GLA_KEY_DIM)),
        'gla_norm': gain((L, GLA_VALUE_DIM // GLA_HEADS)),
        'w_proj_moba': nrm((L, MOBA_WIDTH, D), MOBA_WIDTH),
        'w_proj_gla': nrm((L, GLA_VALUE_DIM, D), GLA_VALUE_DIM),
        'w_out': nrm((L, D, D), D),
        'norm_ffn': gain((L, D)),
        'w_router': nrm((L, D, E), D),
        'b_router': small((L, E)),
        'w_gate': nrm((L, E, D, F), D),
        'b_gate': small((L, E, F)),
        'w_up': nrm((L, E, D, F), D),
        'b_up': small((L, E, F)),
        'w_down': nrm((L, E, F, D), F),
        'b_down': small((L, E, D)),
        'norm_final': gain((D,)),
    }


def reference(x, c, rel_bias, w_ada, b_ada, norm_mix, w_in, w_gk_up, b_gk, gla_norm,
              w_proj_moba, w_proj_gla, w_out, norm_ffn, w_router, b_router,
              w_gate, b_gate, w_up, b_up, w_down, b_down, norm_final):
    B, S, D = x.shape
    split_at = np.cumsum(IN_SIZES)[:-1].tolist()

    def to_heads(t, n):
        return jnp.moveaxis(t.reshape(B, S, n, -1), 2, 1)

    def gla_heads(t):
        return t.reshape(B, S, GLA_HEADS, -1)

    for l in range(DEPTH):
        mod = jax.nn.silu(c) @ w_ada[l] + b_ada[l]
        sh1, sc1, g1, sh2, sc2, g2 = jnp.split(mod, 6, axis=-1)
        h = modulate(rmsnorm(x, norm_mix[l]), sh1, sc1)
        proj = h @ w_in[l]
        qa, ka, va, qb, kb, vb, gk_low, r, gate_logits = jnp.split(proj, split_at, axis=-1)
        y_a = moba_attention(to_heads(qa, MOBA_HEADS), to_heads(ka, MOBA_HEADS),
                             to_heads(va, MOBA_HEADS), rel_bias)
        y_a = jnp.moveaxis(y_a, 1, 2).reshape(B, S, MOBA_WIDTH)
        log_a = jax.nn.log_sigmoid((gk_low @ w_gk_up[l] + b_gk[l]).astype(jnp.float32)) / GLA_GATE_NORMALIZER
        o_b = gla_chunked(gla_heads(qb), gla_heads(kb), gla_heads(vb), gla_heads(log_a))
        y_b = (rmsnorm(o_b, gla_norm[l]) * jax.nn.silu(gla_heads(r))).reshape(B, S, GLA_VALUE_DIM)
        g_a, g_b = jnp.split(jax.nn.sigmoid(gate_logits), N_BRANCHES, axis=-1)
        mixed = g_a * (y_a @ w_proj_moba[l]) + g_b * (y_b @ w_proj_gla[l])
        x = x + g1[:, None, :] * (mixed @ w_out[l])
        h = modulate(rmsnorm(x, norm_ffn[l]), sh2, sc2)
        x = x + g2[:, None, :] * moe_ffn(h, w_router[l], b_router[l], w_gate[l], b_gate[l],
                                         w_up[l], b_up[l], w_down[l], b_down[l])
    return rmsnorm(x, norm_final)
```

```python
import math
from contextlib import ExitStack
import numpy as np
import concourse.bass as bass
import concourse.mybir as mybir
from concourse.bass_utils import run_bass_kernel_spmd

F32 = mybir.dt.float32
BF16 = mybir.dt.bfloat16
I32 = mybir.dt.int32
U32 = mybir.dt.uint32
AF = mybir.ActivationFunctionType
ALU = mybir.AluOpType
AX = mybir.AxisListType

S = 8192
D = 1024
NOWN = 4096
NEG = -30000.0
NSLOT = 24576
NBLK = 96
C_ID, C_TRIU, C_TRIS, C_MASKU, C_USTR, C_ONES, C_IOTAE, C_PIOTA, C_BLKI, C_THR = 0, 128, 256, 384, 512, 640, 768, 800, 808, 904
NCON = 920


class Buf:
    __slots__ = ("name", "w", "r")

    def __init__(self, name):
        self.name = name
        self.w = None
        self.r = {}


class V:
    __slots__ = ("ap", "buf")

    def __init__(self, ap, buf):
        self.ap = ap
        self.buf = buf

    def __getitem__(self, i):
        return V(self.ap[i], self.buf)

    def re(self, pat, **kw):
        return V(self.ap.rearrange(pat, **kw), self.buf)


class Prog:
    NDMA = 64

    def __init__(self, nc):
        self.nc = nc
        self.eng = {"pe": nc.tensor, "act": nc.scalar, "dve": nc.vector, "pool": nc.gpsimd, "sp": nc.sync}
        self.sem = {}
        self.cnt = {}
        for e in self.eng:
            self.sem[e] = nc.alloc_semaphore("s_" + e)
            self.cnt[e] = 0
        self.dsem = []
        for i in range(self.NDMA):
            k = ("d", i)
            self.sem[k] = nc.alloc_semaphore("d%d" % i)
            self.cnt[k] = 0
            self.dsem.append(k)
        self.dnext = 0
        self.known = {e: {} for e in self.eng}
        self.ninstr = 0

    def _wait(self, eng, key, val):
        if val <= 0:
            return
        kn = self.known[eng]
        if kn.get(key, 0) >= val:
            return
        self.eng[eng].wait_ge(self.sem[key], val)
        kn[key] = val

    def _deps(self, eng, reads, writes):
        need = {}
        for b in reads:
            if b.w is not None:
                k, v = b.w
                if need.get(k, 0) < v:
                    need[k] = v
        for b in writes:
            if b.w is not None:
                k, v = b.w
                if need.get(k, 0) < v:
                    need[k] = v
            for k, v in b.r.items():
                if need.get(k, 0) < v:
                    need[k] = v
        for k, v in need.items():
            if k == "pe" and eng == "pe":
                continue
            self._wait(eng, k, v)

    def _mark(self, key, val, reads, writes):
        for b in reads:
            if b.r.get(key, 0) < val:
                b.r[key] = val
        for b in writes:
            b.w = (key, val)
            b.r = {}

    def op(self, eng, fn, reads=(), writes=()):
        self._deps(eng, reads, writes)
        ins = fn(self.eng[eng])
        self.cnt[eng] += 1
        ins.then_inc(self.sem[eng], 1)
        self._mark(eng, self.cnt[eng], reads, writes)
        self.ninstr += 1
        return ins

    def dma(self, eng, fn, reads=(), writes=()):
        self._deps(eng, reads, writes)
        k = self.dsem[self.dnext]
        self.dnext = (self.dnext + 1) % self.NDMA
        self._wait(eng, k, self.cnt[k])
        ins = fn(self.eng[eng])
        self.cnt[k] += 16
        ins.then_inc(self.sem[k], 16)
        self._mark(k, self.cnt[k], reads, writes)
        self.ninstr += 1
        return ins

    def barrier(self):
        for e in self.eng:
            for k in self.sem:
                self._wait(e, k, self.cnt[k])


class Builder:
    def __init__(self, upto="H", debug=()):
        self.upto = upto
        self.debug = set(debug)
        self.nc = bass.Bass("TRN2", target_bir_lowering=False)
        self.P = Prog(self.nc)
        self.psi = 0
        self.nb = 0

    def newbuf(self, name=None):
        self.nb += 1
        return Buf(name or "b%d" % self.nb)

    def sb(self, es, name, shape, dt):
        h = es.enter_context(self.nc.sbuf_tensor(name, list(shape), dt))
        return V(h.ap(), self.newbuf(name))

    def din(self, name, shape, dt):
        return self.nc.dram_tensor(name, list(shape), dt, kind="ExternalInput").ap()

    def dscr(self, name, shape, dt):
        return self.nc.dram_tensor(name, list(shape), dt, kind="Internal").ap()

    def dout(self, name, shape, dt):
        return self.nc.dram_tensor(name, list(shape), dt, kind="ExternalOutput").ap()

    def nps(self):
        p = self.ps[self.psi]
        self.psi = (self.psi + 1) % len(self.ps)
        return p

    def mm(self, out, lhsT, rhs, st=True, sp=True):
        self.P.op("pe", lambda e: e.matmul(out=out.ap, lhsT=lhsT.ap, rhs=rhs.ap, start=st, stop=sp),
                  [lhsT.buf, rhs.buf], [out.buf])

    def tr(self, out, in_, ident):
        self.P.op("pe", lambda e: e.transpose(out=out.ap, in_=in_.ap, identity=ident.ap),
                  [in_.buf, ident.buf], [out.buf])

    def act(self, out, in_, func, bias=None, scale=None, accum=None):
        rd = [in_.buf]
        kw = {}
        if bias is not None:
            if isinstance(bias, V):
                rd.append(bias.buf)
                kw["bias"] = bias.ap
            else:
                kw["bias"] = float(bias)
        if scale is not None:
            if isinstance(scale, V):
                rd.append(scale.buf)
                kw["scale"] = scale.ap
            else:
                kw["scale"] = float(scale)
        wr = [out.buf]
        if accum is not None:
            kw["accum_out"] = accum.ap
            wr.append(accum.buf)
        self.P.op("act", lambda e: e.activation(out=out.ap, in_=in_.ap, func=func, **kw), rd, wr)

    def ts(self, out, in0, s1, s2, op0, op1=None, eng="dve"):
        rd = [in0.buf]
        a1 = s1
        a2 = s2
        if isinstance(s1, V):
            rd.append(s1.buf)
            a1 = s1.ap
        if isinstance(s2, V):
            rd.append(s2.buf)
            a2 = s2.ap
        if op1 is None:
            self.P.op(eng, lambda e: e.tensor_scalar(out=out.ap, in0=in0.ap, scalar1=a1, scalar2=None, op0=op0),
                      rd, [out.buf])
        else:
            self.P.op(eng, lambda e: e.tensor_scalar(out=out.ap, in0=in0.ap, scalar1=a1, scalar2=a2, op0=op0, op1=op1),
                      rd, [out.buf])

    def tt(self, out, in0, in1, op, eng="dve"):
        self.P.op(eng, lambda e: e.tensor_tensor(out=out.ap, in0=in0.ap, in1=in1.ap, op=op),
                  [in0.buf, in1.buf], [out.buf])

    def stt(self, out, in0, sc, in1, op0, op1):
        rd = [in0.buf, in1.buf]
        a = sc
        if isinstance(sc, V):
            rd.append(sc.buf)
            a = sc.ap
        self.P.op("dve", lambda e: e.scalar_tensor_tensor(out=out.ap, in0=in0.ap, scalar=a, in1=in1.ap, op0=op0, op1=op1),
                  rd, [out.buf])

    def cp(self, out, in_, eng="dve"):
        if eng == "act":
            self.P.op("act", lambda e: e.copy(out=out.ap, in_=in_.ap), [in_.buf], [out.buf])
        else:
            self.P.op(eng, lambda e: e.tensor_copy(out=out.ap, in_=in_.ap), [in_.buf], [out.buf])

    def memset(self, out, val, eng="dve"):
        self.P.op(eng, lambda e: e.memset(out.ap, float(val)), [], [out.buf])

    def red(self, out, in_, op=ALU.add, axis=AX.X):
        self.P.op("dve", lambda e: e.tensor_reduce(out=out.ap, in_=in_.ap, axis=axis, op=op), [in_.buf], [out.buf])

    def ld(self, out, src_ap, q="sp", slow=False):
        if slow:
            self.P.dma(q, lambda e: e.dma_start(out=out.ap, in_=src_ap, allow_slow_non_contiguous=True), [], [out.buf])
        else:
            self.P.dma(q, lambda e: e.dma_start(out=out.ap, in_=src_ap), [], [out.buf])

    def st(self, dst_ap, in_, q="sp"):
        self.P.dma(q, lambda e: e.dma_start(out=dst_ap, in_=in_.ap), [in_.buf], [])

    def gather(self, out, src_ap, idx):
        self.P.dma("pool", lambda e: e.indirect_dma_start(out=out.ap, out_offset=None, in_=src_ap,
                                                          in_offset=bass.IndirectOffsetOnAxis(ap=idx.ap, axis=0)),
                   [idx.buf], [out.buf])

    def scatter(self, dst_ap, in_, idx):
        self.P.dma("pool", lambda e: e.indirect_dma_start(out=dst_ap, out_offset=bass.IndirectOffsetOnAxis(ap=idx.ap, axis=0),
                                                          in_=in_.ap, in_offset=None),
                   [idx.buf, in_.buf], [])

    def build(self):
        nc = self.nc
        upto = self.upto
        I = {}
        I["xf"] = self.din("xf", [S, D], F32)
        I["xo"] = self.din("xo", [NOWN, D], F32)
        I["cvec"] = self.din("cvec", [D], F32)
        I["rel31"] = self.din("rel31", [8], F32)
        I["w_ada"] = self.din("w_ada", [D, 6 * D], F32)
        I["b_ada"] = self.din("b_ada", [6 * D], F32)
        I["norm_mix"] = self.din("norm_mix", [D], F32)
        I["w_in"] = self.din("w_in", [D, 6672], F32)
        I["w_gk_up"] = self.din("w_gk_up", [16, 512], F32)
        I["b_gk"] = self.din("b_gk", [512], F32)
        I["gla_norm"] = self.din("gla_norm", [256], F32)
        I["w_pa"] = self.din("w_pa", [512, D], F32)
        I["w_pb"] = self.din("w_pb", [D, D], F32)
        I["w_out"] = self.din("w_out", [D, D], F32)
        I["norm_ffn"] = self.din("norm_ffn", [D], F32)
        I["w_router"] = self.din("w_router", [D, 32], F32)
        I["b_router"] = self.din("b_router", [32], F32)
        if upto >= "G":
            I["w_gate"] = self.din("w_gate", [32 * 128, 8 * D], F32)
            I["w_up"] = self.din("w_up", [32 * 128, 8 * D], F32)
            I["w_down"] = self.din("w_down", [32 * 128, 8 * D], F32)
        I["bgu"] = self.din("bgu", [32, 2048], F32)
        I["b_down"] = self.din("b_down", [32, D], F32)
        I["norm_final"] = self.din("norm_final", [D], F32)
        I["consts"] = self.din("consts", [128, NCON], F32)
        I["gm"] = self.din("gm", [3 * 16 * 32], F32)
        I["btab"] = self.din("btab", [8, 128, 1536], F32)
        I["own_idx"] = self.din("own_idx", [128, 32], I32)
        I["koh"] = self.din("koh", [32, S], F32)
        self.I = I
        out = self.dout("out", [NOWN, D], F32)
        Sx = {}
        Sx["kT"] = self.dscr("kT_s", [512, S], BF16)
        Sx["v"] = self.dscr("v_s", [S, 512], BF16)
        Sx["ob"] = self.dscr("ob_s", [S, D], F32)
        Sx["qT"] = self.dscr("qT_s", [8, 96, NOWN], BF16)
        Sx["yaT"] = self.dscr("yaT_s", [512, NOWN], BF16)
        Sx["x1"] = self.dscr("x1_s", [NOWN, D], F32)
        Sx["h2"] = self.dscr("h2_s", [NOWN, D], BF16)
        Sx["xpad"] = self.dscr("xpad_s", [NSLOT, D], BF16)
        Sx["ypad"] = self.dscr("ypad_s", [NSLOT, D], F32)
        self.Sx = Sx
        self.dbg = {}
        for name, shape, dt in (("d_ob", [S, D], F32), ("d_kT", [512, S], BF16), ("d_v", [S, 512], BF16),
                                ("d_mod", [128, 48], F32), ("d_yaT", [512, NOWN], BF16), ("d_x1", [NOWN, D], F32),
                                ("d_h2", [NOWN, D], BF16), ("d_qT", [8, 96, NOWN], BF16), ("d_logits", [128, 32 * 32], F32),
                                ("d_misc", [128, 2048], F32)):
            if name in self.debug:
                self.dbg[name] = self.dout(name, shape, dt)

        self.ps = [V(nc.alloc_psum_tensor("ps%d" % i, [128, 512], F32).ap(), self.newbuf("ps%d" % i)) for i in range(8)]
        with ExitStack() as top:
            self.top = top
            self.phase_A(top)
            self.P.barrier()
            if upto >= "B":
                self.phase_B()
            if upto >= "C":
                self.phase_C()
            if upto >= "D":
                self.phase_D()
            if upto >= "E":
                self.phase_E()
            if upto >= "F":
                self.phase_F()
            if upto >= "G":
                self.phase_G()
            if upto >= "H":
                self.phase_H(out)
            else:
                with ExitStack() as es:
                    z = self.sb(es, "zout", [128, D], F32)
                    self.memset(z, 0.0)
                    self.st(out[0:128, :], z)
                    self.P.barrier()
            self.debug_copies()
            self.P.barrier()
        return nc

    def debug_copies(self):
        mp = {"d_ob": "ob", "d_kT": "kT", "d_v": "v", "d_yaT": "yaT", "d_x1": "x1", "d_h2": "h2", "d_qT": "qT"}
        with ExitStack() as es:
            for dn, sn in mp.items():
                if dn not in self.dbg:
                    continue
                src = self.Sx[sn]
                dst = self.dbg[dn]
                if len(src.shape) == 3:
                    src = src.rearrange("a b c -> (a b) c")
                    dst = dst.rearrange("a b c -> (a b) c")
                rows, cols = src.shape
                t = self.sb(es, "dbg_" + sn, [128, cols], src.dtype)
                import os
                for r0 in range(0, min(rows, int(os.environ.get("DBGROWS", rows))), 128):
                    self.ld(t, src[r0:r0 + 128, :], q="pool")
                    self.st(dst[r0:r0 + 128, :], t)
            self.P.barrier()

    def phase_A(self, top):
        I = self.I
        cst = self.sb(top, "cst", [128, NCON], F32)
        self.ld(cst, I["consts"])
        self.cst = cst
        self.ident = cst[:, C_ID:C_ID + 128]
        self.triu = cst[:, C_TRIU:C_TRIU + 128]
        self.tris = cst[:, C_TRIS:C_TRIS + 128]
        self.masku = cst[:, C_MASKU:C_MASKU + 128]
        self.onesf = cst[:, C_ONES:C_ONES + 128]
        self.iotae = cst[:, C_IOTAE:C_IOTAE + 32]
        self.piota = cst[:, C_PIOTA:C_PIOTA + 8]
        self.blki = cst[:, C_BLKI:C_BLKI + 96]
        self.thr = cst[:, C_THR:C_THR + 16]
        self.identb = self.sb(top, "identb", [128, 128], BF16)
        self.cp(self.identb, self.ident)
        self.ustrb = self.sb(top, "ustrb", [128, 128], BF16)
        self.cp(self.ustrb, cst[:, C_USTR:C_USTR + 128])
        self.onesb = self.sb(top, "onesb", [128, 128], BF16)
        self.cp(self.onesb, self.onesf)
        self.modT = self.sb(top, "modT", [128, 48], F32)
        self.modbc = self.sb(top, "modbc", [128, 4, D], F32)
        self.A1T = self.sb(top, "A1T", [128, 8], F32)
        self.A2bc = self.sb(top, "A2bc", [128, D], F32)
        self.nfbc = self.sb(top, "nfbc", [128, D], F32)
        self.gnbc = self.sb(top, "gnbc", [128, 256], F32)
        self.c31 = self.sb(top, "c31", [128, 8], F32)
        self.gmt = self.sb(top, "gmt", [128, 3, 16, 32], F32)
        self.kmT = self.sb(top, "kmT", [128, 4, 32], BF16)
        self.kms = self.sb(top, "kms", [128, 4, 32], F32)
        self.ownidx = self.sb(top, "ownidx", [128, 32], I32)
        self.logits = self.sb(top, "logits", [128, 32, 32], F32)
        self.destI = self.sb(top, "destI", [128, 32, 4], I32)
        self.wk = self.sb(top, "wk", [128, 32, 4], F32)
        self.ld(self.nfbc, I["norm_final"].partition_broadcast(128))
        self.ld(self.gnbc, I["gla_norm"].partition_broadcast(128))
        self.ld(self.c31, I["rel31"].partition_broadcast(128))
        self.ld(self.gmt.re("p a b c -> p (a b c)"), I["gm"].partition_broadcast(128))
        self.ld(self.ownidx, I["own_idx"])
        with ExitStack() as es:
            cT = self.sb(es, "cT", [128, 8], F32)
            self.ld(cT, I["cvec"].rearrange("(c p) -> p c", p=128), slow=True)
            sT = self.sb(es, "sT", [128, 8], F32)
            self.act(sT, cT, AF.Silu)
            sTb = self.sb(es, "sTb", [128, 8, 128], F32)
            for c in range(8):
                self.ts(sTb[:, c, :], self.onesf, sT[:, c:c + 1], None, ALU.mult)
            badaT = self.sb(es, "badaT", [128, 48], F32)
            self.ld(badaT, I["b_ada"].rearrange("(c p) -> p c", p=128), slow=True)
            nmT = self.sb(es, "nmT", [128, 8], F32)
            self.ld(nmT, I["norm_mix"].rearrange("(c p) -> p c", p=128), slow=True)
            nffbc = self.sb(es, "nffbc", [128, D], F32)
            self.ld(nffbc, I["norm_ffn"].partition_broadcast(128))
            wa = [self.sb(es, "wa%d" % i, [128, 8, 512], F32) for i in range(2)]
            bb = [self.sb(es, "bb%d" % i, [128, 512], F32) for i in range(2)]
            psB = self.ps[7]
            save = self.ps
            self.ps = self.ps[0:7]
            self.psi = 0
            wsrc = I["w_ada"].rearrange("(c p) n -> p c n", p=128)
            for j in range(12):
                w = wa[j % 2]
                self.ld(w, wsrc[:, :, j * 512:(j + 1) * 512])
                if j >= 4:
                    b_ = bb[j % 2]
                    self.ld(b_, I["b_ada"][j * 512:(j + 1) * 512].partition_broadcast(128))
                    pa = self.nps()
                    for c in range(8):
                        self.mm(pa, sTb[:, c, :], w[:, c, :], st=(c == 0), sp=(c == 7))
                    k = (j - 4) // 2
                    self.tt(self.modbc[:, k, (j % 2) * 512:(j % 2) * 512 + 512], pa, b_, ALU.add)
                for g in range(4):
                    col = j * 4 + g
                    for c in range(8):
                        self.mm(psB[:, col:col + 1], w[:, c, g * 128:(g + 1) * 128], sT[:, c:c + 1], st=(c == 0), sp=(c == 7))
            self.tt(self.modT, psB[:, 0:48], badaT, ALU.add)
            self.ps = save
            self.psi = 0
            self.stt(self.A1T, self.modT[:, 8:16], 1.0, nmT, ALU.add, ALU.mult)
            self.stt(self.A2bc, self.modbc[:, 2, :], 1.0, nffbc, ALU.add, ALU.mult)
            if "d_mod" in self.dbg:
                self.st(self.dbg["d_mod"], self.modT)
            if "d_misc" in self.dbg:
                self.st(self.dbg["d_misc"][:, 0:1024], self.modbc[:, 0, :])
                self.st(self.dbg["d_misc"][:, 1024:2048], self.A2bc)
            self.P.barrier()

    def norm_to_hT(self, xt, hT_slice_fn, scr, AT, BT):
        junk, ss, xn = scr
        self.act(junk, xt, AF.Square, accum=ss)
        self.ts(ss, ss, 1.0 / D, 1e-6, ALU.mult, ALU.add)
        self.act(ss, ss, AF.Sqrt)
        self.P.op("dve", lambda e: e.reciprocal(out=ss.ap, in_=ss.ap), [ss.buf], [ss.buf])
        self.ts(xn, xt, ss[:, 0:1], None, ALU.mult)
        for half in range(2):
            pp = self.nps()
            for cc in range(4):
                c = half * 4 + cc
                self.tr(pp[:, cc * 128:(cc + 1) * 128], xn[:, c * 128:(c + 1) * 128], self.ident)
            for cc in range(4):
                c = half * 4 + cc
                self.act(hT_slice_fn(c), pp[:, cc * 128:(cc + 1) * 128], AF.Identity, bias=BT[:, c:c + 1], scale=AT[:, c:c + 1])

    def phase_B(self):
        I, Sx = self.I, self.Sx
        with ExitStack() as es:
            NCB = 3088
            wB = self.sb(es, "wB", [128, 8, NCB], BF16)
            wsrc = I["w_in"].rearrange("(c p) n -> p c n", p=128)
            import os
            SKIP = os.environ.get("SKIP", "")
            for c in range(0 if "wB" in SKIP else 8):
                self.ld(wB[:, c, :], wsrc[:, c, 512:3600], q="pool")
            wgk = self.sb(es, "wgk", [17, 512], F32)
            if "wgk" not in SKIP:
                self.ld(wgk[0:16, :], I["w_gk_up"])
                self.ld(wgk[16:17, :], I["b_gk"].rearrange("(o n) -> o n", o=1))
            xt = [self.sb(es, "xtB%d" % i, [128, D], F32) for i in range(2)]
            junk = self.sb(es, "junkB", [128, D], F32)
            ss = self.sb(es, "ssB", [128, 1], F32)
            xn = self.sb(es, "xnB", [128, D], F32)
            hT = self.sb(es, "hTB", [128, 8, 512], BF16)
            ktb = [self.sb(es, "ktb%d" % i, [128, 512], BF16) for i in range(2)]
            qbT = self.sb(es, "qbT", [128, 4, 512], F32)
            kbT = self.sb(es, "kbT", [128, 4, 512], F32)
            gkT = self.sb(es, "gkT", [17, 512], F32)
            if "msA" not in SKIP:
                self.memset(gkT, 1.0)
            vat = [self.sb(es, "vat%d" % i, [128, 512], BF16) for i in range(2)]
            kbt = [self.sb(es, "kbt%d" % i, [128, 512], F32) for i in range(4)]
            vbt = [self.sb(es, "vbt%d" % i, [128, D], BF16) for i in range(4)]
            ee = self.sb(es, "eeB", [128, 512], F32)
            ll = self.sb(es, "llB", [128, 512], F32)
            ebT = self.sb(es, "ebT", [128, 512], F32)
            enbT = self.sb(es, "enbT", [128, 512], F32)
            erem = self.sb(es, "erem", [128, 512], F32)
            qg = self.sb(es, "qg", [128, 4, 128], BF16)
            kg = self.sb(es, "kg", [128, 4, 128], BF16)
            kend = self.sb(es, "kend", [128, 512], BF16)
            attb = self.sb(es, "attb", [128, 4, 128], BF16)
            osb = [self.sb(es, "osb%d" % i, [128, D], F32) for i in range(2)]
            St = [self.sb(es, "St%d" % i, [128, 256], F32) for i in range(4)]
            Sb = [self.sb(es, "Sb%d" % i, [128, 256], BF16) for i in range(4)]
            for h in range(0 if "msB" in SKIP else 4):
                self.memset(St[h], 0.0)
                self.memset(Sb[h], 0.0)
            B1T = self.modT[:, 0:8]
            ntile = 0
            import os
            NG = int(os.environ.get("NG", S // 512))
            SKIP = os.environ.get("SKIP", "")
            for G in range(NG):
                t0 = G * 512
                for tt_ in range(4):
                    x_ = xt[ntile % 2]
                    ntile += 1
                    self.ld(x_, I["xf"][t0 + tt_ * 128:t0 + (tt_ + 1) * 128, :])
                    if "norm" not in SKIP:
                        self.norm_to_hT(x_, lambda c, tt_=tt_: hT[:, c, tt_ * 128:(tt_ + 1) * 128], (junk, ss, xn), self.A1T, B1T)
                for gi in range(0 if "ka" in SKIP else 4):
                    pp = self.nps()
                    for c in range(8):
                        self.mm(pp, wB[:, c, gi * 128:(gi + 1) * 128], hT[:, c, :], st=(c == 0), sp=(c == 7))
                    kt_ = ktb[gi % 2]
                    for hb_ in range(2):
                        self.act(kt_[:, hb_ * 256:(hb_ + 1) * 256], pp[:, hb_ * 256:(hb_ + 1) * 256], AF.Identity,
                                 accum=self.kms[:, gi, 2 * G + hb_:2 * G + hb_ + 1])
                    if "stk" not in SKIP:
                        self.st(Sx["kT"][gi * 128:(gi + 1) * 128, t0:t0 + 512], kt_)
                for gi in range(0 if "qbkb" in SKIP else 4):
                    pp = self.nps()
                    for c in range(8):
                        self.mm(pp, wB[:, c, 1024 + gi * 128:1024 + (gi + 1) * 128], hT[:, c, :], st=(c == 0), sp=(c == 7))
                    self.cp(qbT[:, gi, :], pp, eng="act")
                    pp = self.nps()
                    for c in range(8):
                        self.mm(pp, wB[:, c, 1536 + gi * 128:1536 + (gi + 1) * 128], hT[:, c, :], st=(c == 0), sp=(c == 7))
                    self.cp(kbT[:, gi, :], pp)
                if "gk" not in SKIP:
                    pp = self.nps()
                    for c in range(8):
                        self.mm(pp[0:16, :], wB[:, c, 3072:3088], hT[:, c, :], st=(c == 0), sp=(c == 7))
                    self.cp(gkT[0:16, :], pp[0:16, :])
                for tt_ in range(0 if "tm" in SKIP else 4):
                    hs = lambda c: hT[:, c, tt_ * 128:(tt_ + 1) * 128]
                    pp = self.nps()
                    for c in range(8):
                        self.mm(pp, hs(c), wB[:, c, 512:1024], st=(c == 0), sp=(c == 7))
                    va_ = vat[tt_ % 2]
                    self.cp(va_, pp, eng="act")
                    self.st(Sx["v"][t0 + tt_ * 128:t0 + (tt_ + 1) * 128, :], va_)
                    pp = self.nps()
                    for c in range(8):
                        self.mm(pp, hs(c), wB[:, c, 1536:2048], st=(c == 0), sp=(c == 7))
                    self.cp(kbt[tt_], pp)
                    for hf in range(2):
                        pp = self.nps()
                        for c in range(8):
                            self.mm(pp, hs(c), wB[:, c, 2048 + hf * 512:2048 + (hf + 1) * 512], st=(c == 0), sp=(c == 7))
                        self.cp(vbt[tt_][:, hf * 512:(hf + 1) * 512], pp, eng="act")
                for tt_ in range(0 if "gla" in SKIP else 4):
                    sl = slice(tt_ * 128, (tt_ + 1) * 128)
                    pz = self.nps()
                    self.mm(pz, gkT[0:17, sl], wgk[0:17, :])
                    self.act(ee, pz, AF.Exp, scale=-1.0)
                    self.act(ll, ee, AF.Ln, bias=1.0)
                    pr = self.nps()
                    self.mm(pr, self.tris, ll)
                    pbt = self.nps()
                    for h in range(4):
                        self.mm(pbt[:, h * 128:(h + 1) * 128], ll[:, h * 128:(h + 1) * 128], self.triu)
                    self.act(ebT, pbt, AF.Exp)
                    self.act(enbT, pbt, AF.Exp, scale=-1.0)
                    self.act(erem, pr, AF.Exp)
                    self.stt(qg, qbT[:, :, sl], 128.0 ** -0.5, ebT.re("p (h c) -> p h c", h=4), ALU.mult, ALU.mult)
                    self.tt(kg, kbT[:, :, sl], enbT.re("p (h c) -> p h c", h=4), ALU.mult)
                    self.tt(kend, kbt[tt_], erem, ALU.mult)
                    pa = self.nps()
                    for h in range(4):
                        self.mm(pa[:, h * 128:(h + 1) * 128], kg[:, h, :], qg[:, h, :])
                    for h in range(4):
                        self.tt(attb[:, h, :], pa[:, h * 128:(h + 1) * 128], self.masku, ALU.mult)
                    o_ = osb[tt_ % 2]
                    for hp in range(2):
                        po = self.nps()
                        for hh in range(2):
                            h = hp * 2 + hh
                            self.mm(po[:, hh * 256:(hh + 1) * 256], attb[:, h, :], vbt[tt_][:, h * 256:(h + 1) * 256], st=True, sp=False)
                            self.mm(po[:, hh * 256:(hh + 1) * 256], qg[:, h, :], Sb[h], st=False, sp=True)
                        self.cp(o_[:, hp * 512:(hp + 1) * 512], po, eng=("act" if hp else "dve"))
                    self.st(Sx["ob"][t0 + tt_ * 128:t0 + (tt_ + 1) * 128, :], o_)
                    for hp in range(2):
                        pu = self.nps()
                        for hh in range(2):
                            h = hp * 2 + hh
                            self.mm(pu[:, hh * 256:(hh + 1) * 256], kend[:, h * 128:(h + 1) * 128], vbt[tt_][:, h * 256:(h + 1) * 256])
                        for hh in range(2):
                            h = hp * 2 + hh
                            self.stt(St[h], St[h], ebT[:, h * 128 + 127:h * 128 + 128], pu[:, hh * 256:(hh + 1) * 256], ALU.mult, ALU.add)
                            self.cp(Sb[h], St[h], eng="act")
            self.ts(self.kmT, self.kms, 1.0 / 256.0, None, ALU.mult)
            self.P.barrier()

    def phase_C(self):
        I, Sx = self.I, self.Sx
        with ExitStack() as es:
            wq = self.sb(es, "wq", [128, 8, 512], BF16)
            wsrc = I["w_in"].rearrange("(c p) n -> p c n", p=128)
            for c in range(0, 8, 2):
                self.ld(wq[:, c:c + 2, :], wsrc[:, c:c + 2, 0:512], q="pool")
            xt = [self.sb(es, "xtC%d" % i, [128, D], F32) for i in range(2)]
            junk = self.sb(es, "junkC", [128, D], F32)
            ss = self.sb(es, "ssC", [128, 1], F32)
            xn = self.sb(es, "xnC", [128, D], F32)
            hT = self.sb(es, "hTC", [128, 8, 512], BF16)
            qp = [self.sb(es, "qp%d" % i, [128, 512], BF16) for i in range(2)]
            mrow = [self.sb(es, "mrow%d" % i, [96, 512], BF16) for i in range(2)]
            mpad = [self.sb(es, "mpad%d" % i, [128, 96], F32) for i in range(2)]
            for m_ in mpad:
                self.memset(m_, 0.0)
            gmk = self.sb(es, "gmk", [128, 32], F32)
            m8 = self.sb(es, "m8", [128, 8], F32)
            sel = self.sb(es, "sel", [128, 32], F32)
            B1T = self.modT[:, 0:8]
            ntile = 0
            nm = 0
            for G in range(NOWN // 512):
                t0 = G * 512
                for tt_ in range(4):
                    x_ = xt[ntile % 2]
                    ntile += 1
                    self.ld(x_, I["xo"][t0 + tt_ * 128:t0 + (tt_ + 1) * 128, :])
                    self.norm_to_hT(x_, lambda c, tt_=tt_: hT[:, c, tt_ * 128:(tt_ + 1) * 128], (junk, ss, xn), self.A1T, B1T)
                for gi in range(4):
                    pp = self.nps()
                    for c in range(8):
                        self.mm(pp, wq[:, c, gi * 128:(gi + 1) * 128], hT[:, c, :], st=(c == 0), sp=(c == 7))
                    q_ = qp[gi % 2]
                    self.act(q_, pp, AF.Copy, scale=0.125)
                    for hh in range(2):
                        h = gi * 2 + hh
                        hb = hh * 64
                        self.st(Sx["qT"][h, 0:64, t0:t0 + 512], q_[hb:hb + 64, :])
                        mr = mrow[nm % 2]
                        nm += 1
                        for tt_ in range(4):
                            i = 2 * G + tt_ // 2
                            pg = self.nps()
                            self.mm(pg[:, 0:32], q_[hb:hb + 64, tt_ * 128:(tt_ + 1) * 128], self.kmT[hb:hb + 64, gi, :])
                            self.tt(gmk, pg[:, 0:32], self.gmt[:, 0, i, :], ALU.add)
                            self.P.op("dve", lambda e: e.max(out=m8.ap, in_=gmk.ap), [gmk.buf], [m8.buf])
                            self.ts(sel, gmk, m8[:, 2:3], None, ALU.is_ge)
                            self.tt(sel, sel, self.gmt[:, 1, i, :], ALU.mult)
                            self.tt(sel, sel, self.gmt[:, 2, i, :], ALU.add)
                            mp_ = mpad[tt_ % 2]
                            self.ts(mp_[:, 64:96], sel, -1.0, -NEG, ALU.add, ALU.mult)
                            pt = self.nps()
                            self.tr(pt[0:96, 0:128], mp_, self.ident)
                            self.cp(mr[64:96, tt_ * 128:(tt_ + 1) * 128], pt[64:96, 0:128], eng="act")
                        self.st(Sx["qT"][h, 64:96, t0:t0 + 512], mr[64:96, :])
            self.P.barrier()

    def phase_D(self):
        I, Sx = self.I, self.Sx
        with ExitStack() as es:
            kaug = [self.sb(es, "kaug%d" % i, [96, S], BF16) for i in range(2)]
            vaug = [self.sb(es, "vaug%d" % i, [128, 64, 65], BF16) for i in range(2)]
            qaug = [self.sb(es, "qaug%d" % i, [96, NOWN], BF16) for i in range(2)]
            bt = [self.sb(es, "bt%d" % i, [128, 1536], F32) for i in range(2)]
            sbias = [self.sb(es, "sbias%d" % i, [128, 512], F32) for i in range(2)]
            pT = [self.sb(es, "pT%d" % i, [128, 512], BF16) for i in range(3)]
            osb = [self.sb(es, "osbD%d" % i, [65, 256], F32) for i in range(2)]
            rinv = [self.sb(es, "rinv%d" % i, [65, 256], F32) for i in range(2)]
            yo = [self.sb(es, "yo%d" % i, [64, 256], BF16) for i in range(2)]
            for i in range(2):
                self.ld(kaug[i][64:96, :], I["koh"], q="pool")
                self.memset(vaug[i], 1.0)
            vsrc = Sx["v"].rearrange("(t p) f -> p t f", p=128)
            npT = 0
            nsb = 0
            nfin = 0
            psos = [self.ps[6], self.ps[7]]
            save = self.ps
            self.ps = self.ps[0:6]
            self.psi = 0
            state = {"npT": 0, "nsb": 0, "nfin": 0}

            def emit_S(ka, qa, b_, i, j):
                qs = qa[0:96, i * 256:(i + 1) * 256]
                pss = self.nps()
                for kt in range(2):
                    self.mm(pss[:, kt * 256:(kt + 1) * 256], ka[0:96, (2 * j + kt) * 128:(2 * j + kt + 1) * 128], qs)
                src = pss
                if j >= 2 * i - 1:
                    which = j - (2 * i - 1)
                    sb_ = sbias[state["nsb"] % 2]
                    state["nsb"] += 1
                    self.tt(sb_, pss, b_[:, which * 512:(which + 1) * 512], ALU.add)
                    src = sb_
                p_ = pT[state["npT"] % 3]
                state["npT"] += 1
                self.act(p_, src, AF.Exp)
                return p_

            def emit_PV(va, h, i, j, p_):
                nj = 2 * i + 2
                pso = psos[i % 2]
                for kt in range(2):
                    self.mm(pso[0:65, 0:256], va[:, 2 * j + kt, :], p_[:, kt * 256:(kt + 1) * 256],
                            st=(j == 0 and kt == 0), sp=(j == nj - 1 and kt == 1))
                if j == nj - 1:
                    nf = state["nfin"]
                    o_, r_, y_ = osb[nf % 2], rinv[nf % 2], yo[nf % 2]
                    state["nfin"] += 1
                    self.cp(o_, pso[0:65, 0:256])
                    self.P.op("dve", lambda e: e.reciprocal(out=r_[64:65, :].ap, in_=o_[64:65, :].ap), [o_.buf], [r_.buf])
                    pb = self.nps()
                    self.mm(pb[0:64, 0:256], self.onesf[64:65, 0:64], r_[64:65, :])
                    self.tt(y_, pb[0:64, 0:256], o_[0:64, :], ALU.mult)
                    self.st(Sx["yaT"][h * 64:(h + 1) * 64, i * 256:(i + 1) * 256], y_)

            for h in range(8):
                ka, va, qa, b_ = kaug[h % 2], vaug[h % 2], qaug[h % 2], bt[h % 2]
                self.ld(ka[0:64, :], Sx["kT"][h * 64:(h + 1) * 64, :])
                for t8 in range(8):
                    self.ld(va[:, t8 * 8:(t8 + 1) * 8, 0:64], vsrc[:, t8 * 8:(t8 + 1) * 8, h * 64:(h + 1) * 64])
                self.ld(qa, Sx["qT"][h])
                self.ld(b_, I["btab"][h])
                self.ts(b_, b_, self.c31[:, h:h + 1], None, ALU.subtract)
                items = [(i, j) for i in range(16) for j in range(2 * i + 2)]
                prev = None
                for (i, j) in items:
                    p_ = emit_S(ka, qa, b_, i, j)
                    if prev is not None:
                        emit_PV(va, h, *prev)
                    prev = (i, j, p_)
                emit_PV(va, h, *prev)
            self.ps = save
            self.psi = 0
            self.P.barrier()

    def recip(self, t):
        self.P.op("dve", lambda e: e.reciprocal(out=t.ap, in_=t.ap), [t.buf], [t.buf])

    def phase_E(self):
        I, Sx = self.I, self.Sx
        GT = 256
        with ExitStack() as es:
            wsrc = I["w_in"].rearrange("(c p) n -> p c n", p=128)
            wE = self.sb(es, "wE", [128, 8, 3072], BF16)
            for c in range(8):
                self.ld(wE[:, c, :], wsrc[:, c, 3600:6672], q="pool")
            wpa = self.sb(es, "wpa", [64, 8, D], BF16)
            wpas = I["w_pa"].rearrange("(h d) n -> d h n", d=64)
            for h in range(0, 8, 2):
                self.ld(wpa[:, h:h + 2, :], wpas[:, h:h + 2, :], q="pool")
            wpb = self.sb(es, "wpb", [128, 8, D], BF16)
            wpbs = I["w_pb"].rearrange("(c p) n -> p c n", p=128)
            wout = self.sb(es, "wout", [128, 8, D], BF16)
            wouts = I["w_out"].rearrange("(c p) n -> p c n", p=128)
            for c in range(0, 8, 2):
                self.ld(wpb[:, c:c + 2, :], wpbs[:, c:c + 2, :], q="pool")
                self.ld(wout[:, c:c + 2, :], wouts[:, c:c + 2, :], q="pool")
            wr = self.sb(es, "wr", [128, 8, 32], F32)
            self.ld(wr, I["w_router"].rearrange("(c p) n -> p c n", p=128))
            br = self.sb(es, "br", [1, 32], F32)
            self.ld(br, I["b_router"].rearrange("(o n) -> o n", o=1))
            xg = self.sb(es, "xg", [128, 2, D], F32)
            A_ = self.sb(es, "EA", [128, D], F32)
            B_ = self.sb(es, "EB", [128, D], F32)
            C_ = self.sb(es, "EC", [128, D], F32)
            D_ = self.sb(es, "ED", [128, D], F32)
            ss = self.sb(es, "ssE", [128, 1], F32)
            ss4 = self.sb(es, "ss4", [128, 4], F32)
            hT = self.sb(es, "hTE", [128, 8, GT], BF16)
            yaT = self.sb(es, "yaTE", [64, 8, GT], BF16)
            ybT = self.sb(es, "ybTE", [128, 8, GT], BF16)
            mixT = self.sb(es, "mixT", [128, 8, GT], BF16)
            sgA = self.sb(es, "sgA", [128, GT], F32)
            sgB = self.sb(es, "sgB", [128, GT], F32)
            t1 = self.sb(es, "t1E", [128, GT], F32)
            t2 = self.sb(es, "t2E", [128, GT], F32)
            h2b = self.sb(es, "h2b", [128, D], BF16)
            B1T = self.modT[:, 0:8]
            yas = Sx["yaT"].rearrange("(h d) t -> d h t", d=64)
            for G in range(NOWN // GT):
                t0 = G * GT
                for tt_ in range(2):
                    self.ld(xg[:, tt_, :], I["xo"][t0 + tt_ * 128:t0 + (tt_ + 1) * 128, :])
                    self.norm_to_hT(xg[:, tt_, :], lambda c, tt_=tt_: hT[:, c, tt_ * 128:(tt_ + 1) * 128], (A_, ss, B_), self.A1T, B1T)
                self.ld(yaT, yas[:, :, t0:t0 + GT])
                for tt_ in range(2):
                    t = G * 2 + tt_
                    sl = slice(tt_ * 128, (tt_ + 1) * 128)
                    self.gather(A_, Sx["ob"], self.ownidx[:, t:t + 1])
                    for hf in range(2):
                        pp = self.nps()
                        for c in range(8):
                            self.mm(pp, hT[:, c, sl], wE[:, c, hf * 512:(hf + 1) * 512], st=(c == 0), sp=(c == 7))
                        self.act(B_[:, hf * 512:(hf + 1) * 512], pp, AF.Silu)
                    for h in range(4):
                        self.act(D_[:, h * 256:(h + 1) * 256], A_[:, h * 256:(h + 1) * 256], AF.Square, accum=ss4[:, h:h + 1])
                    self.ts(ss4, ss4, 1.0 / 256.0, 1e-6, ALU.mult, ALU.add)
                    self.act(ss4, ss4, AF.Sqrt)
                    self.recip(ss4)
                    for h in range(4):
                        self.stt(C_[:, h * 256:(h + 1) * 256], A_[:, h * 256:(h + 1) * 256], ss4[:, h:h + 1], self.gnbc, ALU.mult, ALU.mult)
                    self.tt(C_, C_, B_, ALU.mult)
                    for half in range(2):
                        pp = self.nps()
                        for cc in range(4):
                            c = half * 4 + cc
                            self.tr(pp[:, cc * 128:(cc + 1) * 128], C_[:, c * 128:(c + 1) * 128], self.ident)
                        self.cp(ybT[:, half * 4:(half + 1) * 4, sl], pp.re("p (a b) -> p a b", a=4), eng=("act" if half else "dve"))
                for dg in range(8):
                    pga = self.nps()
                    for c in range(8):
                        self.mm(pga[:, 0:GT], wE[:, c, 1024 + dg * 128:1024 + (dg + 1) * 128], hT[:, c, :], st=(c == 0), sp=(c == 7))
                    self.act(sgA, pga[:, 0:GT], AF.Sigmoid)
                    pgb = self.nps()
                    for c in range(8):
                        self.mm(pgb[:, 0:GT], wE[:, c, 2048 + dg * 128:2048 + (dg + 1) * 128], hT[:, c, :], st=(c == 0), sp=(c == 7))
                    self.act(sgB, pgb[:, 0:GT], AF.Sigmoid)
                    ppa = self.nps()
                    for h in range(8):
                        self.mm(ppa[:, 0:GT], wpa[0:64, h, dg * 128:(dg + 1) * 128], yaT[0:64, h, :], st=(h == 0), sp=(h == 7))
                    ppb = self.nps()
                    for c in range(8):
                        self.mm(ppb[:, 0:GT], wpb[:, c, dg * 128:(dg + 1) * 128], ybT[:, c, :], st=(c == 0), sp=(c == 7))
                    self.tt(t1, ppa[:, 0:GT], sgA, ALU.mult)
                    self.tt(t2, ppb[:, 0:GT], sgB, ALU.mult)
                    self.tt(mixT[:, dg, :], t1, t2, ALU.add)
                for tt_ in range(2):
                    t = G * 2 + tt_
                    sl = slice(tt_ * 128, (tt_ + 1) * 128)
                    rows = slice(t0 + tt_ * 128, t0 + (tt_ + 1) * 128)
                    for hf in range(2):
                        pp = self.nps()
                        for c in range(8):
                            self.mm(pp, mixT[:, c, sl], wout[:, c, hf * 512:(hf + 1) * 512], st=(c == 0), sp=(c == 7))
                        self.tt(A_[:, hf * 512:(hf + 1) * 512], pp, self.modbc[:, 0, hf * 512:(hf + 1) * 512], ALU.mult)
                    self.tt(A_, A_, xg[:, tt_, :], ALU.add)
                    self.st(Sx["x1"][rows, :], A_)
                    self.act(B_, A_, AF.Square, accum=ss)
                    self.ts(ss, ss, 1.0 / D, 1e-6, ALU.mult, ALU.add)
                    self.act(ss, ss, AF.Sqrt)
                    self.recip(ss)
                    self.ts(B_, A_, ss[:, 0:1], None, ALU.mult)
                    self.tt(C_, B_, self.A2bc, ALU.mult)
                    self.tt(C_, C_, self.modbc[:, 1, :], ALU.add)
                    self.cp(h2b, C_, eng="act")
                    self.st(Sx["h2"][rows, :], h2b)
                    for half in range(2):
                        pp = self.nps()
                        for cc in range(4):
                            c = half * 4 + cc
                            self.tr(pp[:, cc * 128:(cc + 1) * 128], C_[:, c * 128:(c + 1) * 128], self.ident)
                        self.cp(D_[:, half * 512:(half + 1) * 512], pp, eng=("act" if half else "dve"))
                    pl = self.nps()
                    for c in range(8):
                        self.mm(pl[:, 0:32], D_[:, c * 128:(c + 1) * 128], wr[:, c, :], st=(c == 0), sp=False)
                    self.mm(pl[:, 0:32], self.onesf[0:1, 0:128], br[0:1, :], st=False, sp=True)
                    self.cp(self.logits[:, t, :], pl[:, 0:32])
            if "d_logits" in self.dbg:
                self.st(self.dbg["d_logits"], self.logits.re("p a b -> p (a b)"))
            self.P.barrier()

    def phase_F(self):
        I, Sx = self.I, self.Sx
        top = self.top
        self.widx = self.sb(top, "widx", [128, NBLK], I32)
        self.bgT = self.sb(top, "bgT", [128, 16, NBLK], F32)
        self.ohT = self.sb(top, "ohT", [32, NBLK], F32)
        self.bdh = self.sb(top, "bdh", [32, D], BF16)
        self.bdl = self.sb(top, "bdl", [32, D], BF16)
        with ExitStack() as es:
            M = self.sb(es, "Moh", [128, 32, 32], BF16)
            m8 = self.sb(es, "m8F", [128, 8], F32)
            idx8 = self.sb(es, "idx8", [128, 8], U32)
            idxall = self.sb(es, "idxall", [128, 32, 4], F32)
            e4 = self.sb(es, "e4", [128, 4], F32)
            s1 = self.sb(es, "s1F", [128, 1], F32)
            negm = self.sb(es, "negm", [128, 1], F32)
            rank = self.sb(es, "rank", [128, 32, 32], F32)
            base = self.sb(es, "base", [128, 32], F32)
            nblk = self.sb(es, "nblk", [128, 32], F32)
            ps_ = self.sb(es, "pstart", [128, 33], F32)
            oh = self.sb(es, "ohF", [128, 32], F32)
            junk = self.sb(es, "junkF", [128, 32], F32)
            destf = self.sb(es, "destf", [128, 32, 4], F32)
            acc = self.sb(es, "accF", [128, NBLK], F32)
            be = self.sb(es, "beF", [128, NBLK], F32)
            widxf = self.sb(es, "widxf", [128, NBLK], F32)
            bgu = self.sb(es, "bguF", [32, 2048], F32)
            bd = self.sb(es, "bdF", [32, D], F32)
            bd2 = self.sb(es, "bd2F", [32, D], F32)
            h2t = [self.sb(es, "h2t%d" % i, [128, D], BF16) for i in range(2)]
            self.ld(bgu, I["bgu"])
            self.ld(bd, I["b_down"])
            for t in range(32):
                lg = self.logits[:, t, :]
                self.P.op("dve", lambda e: e.max(out=m8.ap, in_=lg.ap), [lg.buf], [m8.buf])
                self.P.op("dve", lambda e: e.max_index(out=idx8.ap, in_max=m8.ap, in_values=lg.ap), [m8.buf, lg.buf], [idx8.buf])
                self.cp(idxall[:, t, :], idx8[:, 0:4])
                self.ts(M[:, t, :], lg, m8[:, 3:4], None, ALU.is_ge)
                self.ts(negm, m8[:, 0:1], -1.0, None, ALU.mult)
                self.act(e4, m8[:, 0:4], AF.Exp, bias=negm[:, 0:1], accum=s1)
                self.recip(s1)
                self.ts(self.wk[:, t, :], e4, s1[:, 0:1], None, ALU.mult)
            self.memset(base, 0.0)
            for t in range(32):
                pr = self.nps()
                self.mm(pr[:, 0:32], self.ustrb, M[:, t, :])
                self.mm(pr[:, 32:64], self.onesb, M[:, t, :])
                self.tt(rank[:, t, :], pr[:, 0:32], base, ALU.add)
                self.tt(base, base, pr[:, 32:64], ALU.add)
            self.memset(nblk, 0.0)
            for m in range(16):
                self.stt(nblk, base, self.thr[:, m:m + 1], nblk, ALU.is_gt, ALU.add)
            self.memset(ps_, 0.0)
            for e_ in range(1, 33):
                self.tt(ps_[:, e_:e_ + 1], ps_[:, e_ - 1:e_], nblk[:, e_ - 1:e_], ALU.add)
            for t in range(32):
                self.stt(rank[:, t, :], ps_[:, 0:32], 256.0, rank[:, t, :], ALU.mult, ALU.add)
            for t in range(32):
                for k in range(4):
                    self.ts(oh, self.iotae, idxall[:, t, k:k + 1], None, ALU.is_equal)
                    self.tt(oh, oh, rank[:, t, :], ALU.mult)
                    self.act(junk, oh, AF.Identity, accum=destf[:, t, k:k + 1])
            self.cp(self.destI, destf)
            self.memset(acc, 0.0)
            for e_ in range(32):
                self.stt(acc, self.blki, ps_[:, e_ + 1:e_ + 2], acc, ALU.is_ge, ALU.add)
            self.ts(be, acc, 31.0, None, ALU.min)
            self.ts(widxf, be, 128.0, self.piota[:, 0:1], ALU.mult, ALU.add)
            self.cp(self.widx, widxf)
            self.ts(self.ohT, be[0:32, :], self.piota[0:32, 0:1], None, ALU.is_equal)
            for ft in range(16):
                pp = self.nps()
                self.mm(pp[:, 0:NBLK], bgu[0:32, ft * 128:(ft + 1) * 128], self.ohT[0:32, :])
                self.cp(self.bgT[:, ft, :], pp[:, 0:NBLK])
            self.cp(self.bdh, bd)
            self.cp(bd2, self.bdh)
            self.tt(self.bdl, bd, bd2, ALU.subtract)
            for t in range(32):
                h_ = h2t[t % 2]
                self.ld(h_, Sx["h2"][t * 128:(t + 1) * 128, :])
                for k in range(4):
                    self.scatter(Sx["xpad"], h_, self.destI[:, t, k:k + 1])
            if "d_misc" in self.dbg:
                self.st(self.dbg["d_misc"][:, 0:NBLK], be)
                self.st(self.dbg["d_misc"][:, 128:256], destf.re("p a b -> p (a b)"))
                self.st(self.dbg["d_misc"][:, 256:384], self.wk.re("p a b -> p (a b)"))
                self.st(self.dbg["d_misc"][:, 384:416], base)
            self.P.barrier()

    def phase_G(self):
        I, Sx = self.I, self.Sx
        import os
        NB = int(os.environ.get("NBG", NBLK))
        with ExitStack() as es:
            xb = [self.sb(es, "xb%d" % i, [128, 2, D], BF16) for i in range(2)]
            xT = self.sb(es, "xTG", [128, 8, 256], BF16)
            wg = [self.sb(es, "wg%d" % i, [128, 8, D], BF16) for i in range(2)]
            wu = [self.sb(es, "wu%d" % i, [128, 8, D], BF16) for i in range(2)]
            wd = [self.sb(es, "wd%d" % i, [128, 8, D], BF16) for i in range(2)]
            gm = [self.sb(es, "gmG%d" % i, [128, 256], F32) for i in range(2)]
            sg = [self.sb(es, "sgG%d" % i, [128, 256], F32) for i in range(2)]
            u1 = [self.sb(es, "u1G%d" % i, [128, 256], F32) for i in range(2)]
            actT = self.sb(es, "actT", [128, 8, 256], BF16)
            ohb = [self.sb(es, "ohb%d" % i, [32, 128], BF16) for i in range(2)]
            yb = [self.sb(es, "ybG%d" % i, [128, 2, D], F32) for i in range(2)]
            xsrc = Sx["xpad"].rearrange("(b a p) n -> b p a n", a=2, p=128)
            ydst = Sx["ypad"].rearrange("(b a p) n -> b p a n", a=2, p=128)
            n2 = 0
            for blk in range(NB):
                x_ = xb[blk % 2]
                wg_, wu_, wd_ = wg[blk % 2], wu[blk % 2], wd[blk % 2]
                self.ld(x_, xsrc[blk])
                self.gather(wg_.re("p c n -> p (c n)"), I["w_gate"], self.widx[:, blk:blk + 1])
                self.gather(wu_.re("p c n -> p (c n)"), I["w_up"], self.widx[:, blk:blk + 1])
                self.gather(wd_.re("p c n -> p (c n)"), I["w_down"], self.widx[:, blk:blk + 1])
                for a in range(2):
                    for half in range(2):
                        pp = self.nps()
                        ppb = V(pp.ap.bitcast(BF16), pp.buf)
                        for cc in range(4):
                            c = half * 4 + cc
                            self.tr(ppb[:, cc * 128:(cc + 1) * 128], x_[:, a, c * 128:(c + 1) * 128], self.identb)
                        self.cp(xT[:, half * 4:(half + 1) * 4, a * 128:(a + 1) * 128], ppb[:, 0:512].re("p (a b) -> p a b", a=4),
                                eng=("act" if half else "dve"))
                oh_ = ohb[blk % 2]
                self.ts(oh_, self.onesf[0:32, 0:128], self.ohT[0:32, blk:blk + 1], None, ALU.mult)
                for ft in range(8):
                    g_, s_, u_ = gm[n2 % 2], sg[n2 % 2], u1[n2 % 2]
                    n2 += 1
                    pg = self.nps()
                    for c in range(8):
                        self.mm(pg[:, 0:256], wg_[:, c, ft * 128:(ft + 1) * 128], xT[:, c, :], st=(c == 0), sp=(c == 7))
                    pu = self.nps()
                    for c in range(8):
                        self.mm(pu[:, 0:256], wu_[:, c, ft * 128:(ft + 1) * 128], xT[:, c, :], st=(c == 0), sp=(c == 7))
                    self.ts(g_, pg[:, 0:256], self.bgT[:, ft, blk:blk + 1], 7.0, ALU.add, ALU.min)
                    self.act(s_, g_, AF.Sigmoid, scale=1.702)
                    self.ts(u_, pu[:, 0:256], self.bgT[:, 8 + ft, blk:blk + 1], 7.0, ALU.add, ALU.min)
                    self.ts(u_, u_, -7.0, 1.0, ALU.max, ALU.add)
                    self.tt(g_, g_, s_, ALU.mult)
                    self.tt(actT[:, ft, :], g_, u_, ALU.mult)
                y_ = yb[blk % 2]
                for a in range(2):
                    for dh in range(2):
                        py = self.nps()
                        for ft in range(8):
                            self.mm(py, actT[:, ft, a * 128:(a + 1) * 128], wd_[:, ft, dh * 512:(dh + 1) * 512], st=(ft == 0), sp=False)
                        self.mm(py, oh_[0:32, :], self.bdh[0:32, dh * 512:(dh + 1) * 512], st=False, sp=False)
                        self.mm(py, oh_[0:32, :], self.bdl[0:32, dh * 512:(dh + 1) * 512], st=False, sp=True)
                        self.cp(y_[:, a, dh * 512:(dh + 1) * 512], py, eng=("act" if dh else "dve"))
                self.st(ydst[blk], y_)
            self.P.barrier()

    def phase_H(self, out):
        I, Sx = self.I, self.Sx
        with ExitStack() as es:
            x1t = [self.sb(es, "x1t%d" % i, [128, D], F32) for i in range(2)]
            yk = [self.sb(es, "yk%d" % i, [128, D], F32) for i in range(8)]
            acc = [self.sb(es, "accH%d" % i, [128, D], F32) for i in range(2)]
            junk = self.sb(es, "junkH", [128, D], F32)
            ss = self.sb(es, "ssH", [128, 1], F32)
            o_ = [self.sb(es, "oH%d" % i, [128, D], F32) for i in range(2)]
            for t in range(32):
                rows = slice(t * 128, (t + 1) * 128)
                x_ = x1t[t % 2]
                a_ = acc[t % 2]
                self.ld(x_, Sx["x1"][rows, :])
                ys = [yk[(t % 2) * 4 + k] for k in range(4)]
                for k in range(4):
                    self.gather(ys[k], Sx["ypad"], self.destI[:, t, k:k + 1])
                self.ts(a_, ys[0], self.wk[:, t, 0:1], None, ALU.mult)
                for k in range(1, 4):
                    self.stt(a_, ys[k], self.wk[:, t, k:k + 1], a_, ALU.mult, ALU.add)
                self.tt(a_, a_, self.modbc[:, 3, :], ALU.mult)
                self.tt(a_, a_, x_, ALU.add)
                self.act(junk, a_, AF.Square, accum=ss)
                self.ts(ss, ss, 1.0 / D, 1e-6, ALU.mult, ALU.add)
                self.act(ss, ss, AF.Sqrt)
                self.recip(ss)
                oo = o_[t % 2]
                self.stt(oo, a_, ss[:, 0:1], self.nfbc, ALU.mult, ALU.mult)
                self.st(out[rows, :], oo)
            self.P.barrier()


def _bucket(n):
    n = np.maximum(n, 0)
    me = 16
    nf = np.maximum(n, me).astype(np.float32)
    large = me + (np.log(nf / me) / math.log(128 / me) * (32 - me)).astype(np.int32)
    large = np.minimum(large, 31)
    return np.where(n < me, n, large)


def make_consts():
    c = np.zeros((128, NCON), np.float32)
    p = np.arange(128)[:, None]
    f = np.arange(128)[None, :]
    c[:, C_ID:C_ID + 128] = (p == f)
    c[:, C_TRIU:C_TRIU + 128] = np.where(p <= f, -1.0 / 16.0, 0.0)
    c[:, C_TRIS:C_TRIS + 128] = np.where(p > f, -1.0 / 16.0, 0.0)
    c[:, C_MASKU:C_MASKU + 128] = (p <= f)
    c[:, C_USTR:C_USTR + 128] = (p < f)
    c[:, C_ONES:C_ONES + 128] = 1.0
    c[:, C_IOTAE:C_IOTAE + 32] = np.arange(32)[None, :]
    c[:, C_PIOTA:C_PIOTA + 8] = p + 128 * np.arange(8)[None, :]
    c[:, C_BLKI:C_BLKI + 96] = np.arange(96)[None, :]
    c[:, C_THR:C_THR + 16] = 256.0 * np.arange(16)[None, :]
    return c


def make_core_tables(p, rel_bias):
    gm = np.zeros((3, 16, 32), np.float32)
    for i in range(16):
        g = 2 * i + p
        j = np.arange(32)
        gm[0, i] = np.where(j < g, 0.0, -1e30)
        gm[1, i] = (j < g)
        gm[2, i] = (j == g)
    k = np.arange(256)[:, None]
    q = np.arange(256)[None, :]
    idx = np.zeros((3, 256, 256), np.int64)
    for which in range(3):
        delta = p + 1 - which
        if delta >= 2:
            idx[which] = 31
        elif delta == 1:
            idx[which] = _bucket(256 + q - k)
        elif delta == 0:
            idx[which] = np.where(k <= q, _bucket(q - k), 32)
        else:
            idx[which] = 32
    btab = np.zeros((8, 128, 3, 2, 256), np.float32)
    for h in range(8):
        ext = np.concatenate([rel_bias[:, h], np.array([NEG], np.float32)]).astype(np.float32)
        t = ext[idx]
        btab[h] = t.reshape(3, 2, 128, 256).transpose(2, 0, 1, 3)
    own_rows = np.concatenate([np.arange((2 * i + p) * 256, (2 * i + p + 1) * 256) for i in range(16)]).astype(np.int32)
    own_idx = np.ascontiguousarray(own_rows.reshape(32, 128).T)
    return gm.reshape(-1), btab.reshape(8, 128, 1536), own_idx, own_rows


def make_in_maps(inputs, cores, upto="H"):
    x = np.asarray(inputs["x"], np.float32)
    consts = make_consts()
    koh = np.zeros((32, S), np.float32)
    for j in range(32):
        koh[j, j * 256:(j + 1) * 256] = 1.0
    g = lambda n: np.ascontiguousarray(np.asarray(inputs[n], np.float32)[0])
    shared = {
        "rel31": np.ascontiguousarray(np.asarray(inputs["rel_bias"], np.float32)[31]),
        "w_ada": g("w_ada"), "b_ada": g("b_ada"), "norm_mix": g("norm_mix"), "w_in": g("w_in"),
        "w_gk_up": g("w_gk_up"), "b_gk": g("b_gk"), "gla_norm": g("gla_norm"), "w_pa": g("w_proj_moba"),
        "w_pb": g("w_proj_gla"), "w_out": g("w_out"), "norm_ffn": g("norm_ffn"), "w_router": g("w_router"),
        "b_router": g("b_router"),
        "bgu": np.ascontiguousarray(np.concatenate([g("b_gate"), g("b_up")], axis=1)),
        "b_down": g("b_down"), "norm_final": np.asarray(inputs["norm_final"], np.float32),
        "consts": consts, "koh": koh,
    }
    if upto >= "G":
        perm = lambda n: np.ascontiguousarray(g(n).reshape(32, 8, 128, D).transpose(0, 2, 1, 3)).reshape(32 * 128, 8 * D)
        shared["w_gate"] = perm("w_gate")
        shared["w_up"] = perm("w_up")
        shared["w_down"] = perm("w_down")
    maps = []
    rows = []
    rb = np.asarray(inputs["rel_bias"], np.float32)
    for core in cores:
        b, p = core // 2, core % 2
        gm, btab, own_idx, own_rows = make_core_tables(p, rb)
        m = dict(shared)
        m["xf"] = np.ascontiguousarray(x[b])
        m["xo"] = np.ascontiguousarray(x[b][own_rows])
        m["cvec"] = np.ascontiguousarray(np.asarray(inputs["c"], np.float32)[b])
        m["gm"] = gm
        m["btab"] = btab
        m["own_idx"] = own_idx
        maps.append(m)
        rows.append((b, own_rows))
    return maps, rows


_NC_CACHE = {}


def kernel(**inputs):
    if "H" not in _NC_CACHE:
        _NC_CACHE["H"] = Builder("H").build()
    nc = _NC_CACHE["H"]
    cores = list(range(8))
    maps, rows = make_in_maps(inputs, cores, "H")
    res = run_bass_kernel_spmd(nc, maps, core_ids=cores)
    outp = np.zeros((4, S, D), np.float32)
    for ci, (b, own_rows) in enumerate(rows):
        outp[b][own_rows] = res.results[ci]["out"]
    return outp
```

```python
import math
from contextlib import ExitStack
import numpy as np
import concourse.bass as bass
import concourse.mybir as mybir
from concourse.bass_utils import run_bass_kernel_spmd

F32 = mybir.dt.float32
BF16 = mybir.dt.bfloat16
I32 = mybir.dt.int32
U32 = mybir.dt.uint32
AF = mybir.ActivationFunctionType
ALU = mybir.AluOpType
AX = mybir.AxisListType

S = 8192
D = 1024
NOWN = 4096
NEG = -30000.0
NSLOT = 24576
NBLK = 96
C_ID, C_TRIU, C_TRIS, C_MASKU, C_USTR, C_ONES, C_IOTAE, C_PIOTA, C_BLKI, C_THR = 0, 128, 256, 384, 512, 640, 768, 800, 808, 904
NCON = 920


class Buf:
    __slots__ = ("name", "w", "r")

    def __init__(self, name):
        self.name = name
        self.w = None
        self.r = {}


class V:
    __slots__ = ("ap", "buf")

    def __init__(self, ap, buf):
        self.ap = ap
        self.buf = buf

    def __getitem__(self, i):
        return V(self.ap[i], self.buf)

    def re(self, pat, **kw):
        return V(self.ap.rearrange(pat, **kw), self.buf)


class Prog:
    NDMA = 64

    def __init__(self, nc):
        self.nc = nc
        self.eng = {"pe": nc.tensor, "act": nc.scalar, "dve": nc.vector, "pool": nc.gpsimd, "sp": nc.sync}
        self.sem = {}
        self.cnt = {}
        for e in self.eng:
            self.sem[e] = nc.alloc_semaphore("s_" + e)
            self.cnt[e] = 0
        self.dsem = []
        for i in range(self.NDMA):
            k = ("d", i)
            self.sem[k] = nc.alloc_semaphore("d%d" % i)
            self.cnt[k] = 0
            self.dsem.append(k)
        self.dnext = 0
        self.known = {e: {} for e in self.eng}
        self.ninstr = 0

    def _wait(self, eng, key, val):
        if val <= 0:
            return
        kn = self.known[eng]
        if kn.get(key, 0) >= val:
            return
        self.eng[eng].wait_ge(self.sem[key], val)
        kn[key] = val

    def _deps(self, eng, reads, writes):
        need = {}
        for b in reads:
            if b.w is not None:
                k, v = b.w
                if need.get(k, 0) < v:
                    need[k] = v
        for b in writes:
            if b.w is not None:
                k, v = b.w
                if need.get(k, 0) < v:
                    need[k] = v
            for k, v in b.r.items():
                if need.get(k, 0) < v:
                    need[k] = v
        for k, v in need.items():
            if k == "pe" and eng == "pe":
                continue
            self._wait(eng, k, v)

    def _mark(self, key, val, reads, writes):
        for b in reads:
            if b.r.get(key, 0) < val:
                b.r[key] = val
        for b in writes:
            b.w = (key, val)
            b.r = {}

    def op(self, eng, fn, reads=(), writes=()):
        self._deps(eng, reads, writes)
        ins = fn(self.eng[eng])
        self.cnt[eng] += 1
        ins.then_inc(self.sem[eng], 1)
        self._mark(eng, self.cnt[eng], reads, writes)
        self.ninstr += 1
        return ins

    def dma(self, eng, fn, reads=(), writes=()):
        self._deps(eng, reads, writes)
        k = self.dsem[self.dnext]
        self.dnext = (self.dnext + 1) % self.NDMA
        self._wait(eng, k, self.cnt[k])
        ins = fn(self.eng[eng])
        self.cnt[k] += 16
        ins.then_inc(self.sem[k], 16)
        self._mark(k, self.cnt[k], reads, writes)
        self.ninstr += 1
        return ins

    def barrier(self):
        for e in self.eng:
            for k in self.sem:
                self._wait(e, k, self.cnt[k])


class Builder:
    def __init__(self, upto="H", debug=()):
        self.upto = upto
        self.debug = set(debug)
        self.nc = bass.Bass("TRN2", target_bir_lowering=False)
        self.P = Prog(self.nc)
        self.psi = 0
        self.nb = 0

    def newbuf(self, name=None):
        self.nb += 1
        return Buf(name or "b%d" % self.nb)

    def sb(self, es, name, shape, dt):
        h = es.enter_context(self.nc.sbuf_tensor(name, list(shape), dt))
        return V(h.ap(), self.newbuf(name))

    def din(self, name, shape, dt):
        return self.nc.dram_tensor(name, list(shape), dt, kind="ExternalInput").ap()

    def dscr(self, name, shape, dt):
        return self.nc.dram_tensor(name, list(shape), dt, kind="Internal").ap()

    def dout(self, name, shape, dt):
        return self.nc.dram_tensor(name, list(shape), dt, kind="ExternalOutput").ap()

    def nps(self):
        p = self.ps[self.psi]
        self.psi = (self.psi + 1) % len(self.ps)
        return p

    def mm(self, out, lhsT, rhs, st=True, sp=True):
        self.P.op("pe", lambda e: e.matmul(out=out.ap, lhsT=lhsT.ap, rhs=rhs.ap, start=st, stop=sp),
                  [lhsT.buf, rhs.buf], [out.buf])

    def tr(self, out, in_, ident):
        self.P.op("pe", lambda e: e.transpose(out=out.ap, in_=in_.ap, identity=ident.ap),
                  [in_.buf, ident.buf], [out.buf])

    def act(self, out, in_, func, bias=None, scale=None, accum=None):
        rd = [in_.buf]
        kw = {}
        if bias is not None:
            if isinstance(bias, V):
                rd.append(bias.buf)
                kw["bias"] = bias.ap
            else:
                kw["bias"] = float(bias)
        if scale is not None:
            if isinstance(scale, V):
                rd.append(scale.buf)
                kw["scale"] = scale.ap
            else:
                kw["scale"] = float(scale)
        wr = [out.buf]
        if accum is not None:
            kw["accum_out"] = accum.ap
            wr.append(accum.buf)
        self.P.op("act", lambda e: e.activation(out=out.ap, in_=in_.ap, func=func, **kw), rd, wr)

    def ts(self, out, in0, s1, s2, op0, op1=None, eng="dve"):
        rd = [in0.buf]
        a1 = s1
        a2 = s2
        if isinstance(s1, V):
            rd.append(s1.buf)
            a1 = s1.ap
        if isinstance(s2, V):
            rd.append(s2.buf)
            a2 = s2.ap
        if op1 is None:
            self.P.op(eng, lambda e: e.tensor_scalar(out=out.ap, in0=in0.ap, scalar1=a1, scalar2=None, op0=op0),
                      rd, [out.buf])
        else:
            self.P.op(eng, lambda e: e.tensor_scalar(out=out.ap, in0=in0.ap, scalar1=a1, scalar2=a2, op0=op0, op1=op1),
                      rd, [out.buf])

    def tt(self, out, in0, in1, op, eng="dve"):
        self.P.op(eng, lambda e: e.tensor_tensor(out=out.ap, in0=in0.ap, in1=in1.ap, op=op),
                  [in0.buf, in1.buf], [out.buf])

    def stt(self, out, in0, sc, in1, op0, op1):
        rd = [in0.buf, in1.buf]
        a = sc
        if isinstance(sc, V):
            rd.append(sc.buf)
            a = sc.ap
        self.P.op("dve", lambda e: e.scalar_tensor_tensor(out=out.ap, in0=in0.ap, scalar=a, in1=in1.ap, op0=op0, op1=op1),
                  rd, [out.buf])

    def cp(self, out, in_, eng="dve"):
        if eng == "act":
            self.P.op("act", lambda e: e.copy(out=out.ap, in_=in_.ap), [in_.buf], [out.buf])
        else:
            self.P.op(eng, lambda e: e.tensor_copy(out=out.ap, in_=in_.ap), [in_.buf], [out.buf])

    def memset(self, out, val, eng="dve"):
        self.P.op(eng, lambda e: e.memset(out.ap, float(val)), [], [out.buf])

    def red(self, out, in_, op=ALU.add, axis=AX.X):
        self.P.op("dve", lambda e: e.tensor_reduce(out=out.ap, in_=in_.ap, axis=axis, op=op), [in_.buf], [out.buf])

    def ld(self, out, src_ap, q="sp", slow=False):
        if slow:
            self.P.dma(q, lambda e: e.dma_start(out=out.ap, in_=src_ap, allow_slow_non_contiguous=True), [], [out.buf])
        else:
            self.P.dma(q, lambda e: e.dma_start(out=out.ap, in_=src_ap), [], [out.buf])

    def st(self, dst_ap, in_, q="sp"):
        self.P.dma(q, lambda e: e.dma_start(out=dst_ap, in_=in_.ap), [in_.buf], [])

    def gather(self, out, src_ap, idx):
        self.P.dma("pool", lambda e: e.indirect_dma_start(out=out.ap, out_offset=None, in_=src_ap,
                                                          in_offset=bass.IndirectOffsetOnAxis(ap=idx.ap, axis=0)),
                   [idx.buf], [out.buf])

    def scatter(self, dst_ap, in_, idx):
        self.P.dma("pool", lambda e: e.indirect_dma_start(out=dst_ap, out_offset=bass.IndirectOffsetOnAxis(ap=idx.ap, axis=0),
                                                          in_=in_.ap, in_offset=None),
                   [idx.buf, in_.buf], [])

    def build(self):
        nc = self.nc
        upto = self.upto
        I = {}
        I["xf"] = self.din("xf", [S, D], F32)
        I["xo"] = self.din("xo", [NOWN, D], F32)
        I["cvec"] = self.din("cvec", [D], F32)
        I["rel31"] = self.din("rel31", [8], F32)
        I["w_ada"] = self.din("w_ada", [D, 6 * D], F32)
        I["b_ada"] = self.din("b_ada", [6 * D], F32)
        I["norm_mix"] = self.din("norm_mix", [D], F32)
        I["w_in"] = self.din("w_in", [D, 6672], F32)
        I["w_gk_up"] = self.din("w_gk_up", [16, 512], F32)
        I["b_gk"] = self.din("b_gk", [512], F32)
        I["gla_norm"] = self.din("gla_norm", [256], F32)
        I["w_pa"] = self.din("w_pa", [512, D], F32)
        I["w_pb"] = self.din("w_pb", [D, D], F32)
        I["w_out"] = self.din("w_out", [D, D], F32)
        I["norm_ffn"] = self.din("norm_ffn", [D], F32)
        I["w_router"] = self.din("w_router", [D, 32], F32)
        I["b_router"] = self.din("b_router", [32], F32)
        if upto >= "G":
            I["w_gate"] = self.din("w_gate", [32 * 128, 8 * D], F32)
            I["w_up"] = self.din("w_up", [32 * 128, 8 * D], F32)
            I["w_down"] = self.din("w_down", [32 * 128, 8 * D], F32)
        I["bgu"] = self.din("bgu", [32, 2048], F32)
        I["b_down"] = self.din("b_down", [32, D], F32)
        I["norm_final"] = self.din("norm_final", [D], F32)
        I["consts"] = self.din("consts", [128, NCON], F32)
        I["gm"] = self.din("gm", [3 * 16 * 32], F32)
        I["btab"] = self.din("btab", [8, 128, 1536], F32)
        I["own_idx"] = self.din("own_idx", [128, 32], I32)
        I["koh"] = self.din("koh", [32, S], F32)
        self.I = I
        out = self.dout("out", [NOWN, D], F32)
        Sx = {}
        Sx["kT"] = self.dscr("kT_s", [512, S], BF16)
        Sx["v"] = self.dscr("v_s", [S, 512], BF16)
        Sx["ob"] = self.dscr("ob_s", [S, D], F32)
        Sx["qT"] = self.dscr("qT_s", [8, 96, NOWN], BF16)
        Sx["yaT"] = self.dscr("yaT_s", [512, NOWN], BF16)
        Sx["x1"] = self.dscr("x1_s", [NOWN, D], F32)
        Sx["h2"] = self.dscr("h2_s", [NOWN, D], BF16)
        Sx["xpad"] = self.dscr("xpad_s", [NSLOT, D], BF16)
        Sx["ypad"] = self.dscr("ypad_s", [NSLOT, D], F32)
        self.Sx = Sx
        self.dbg = {}
        for name, shape, dt in (("d_ob", [S, D], F32), ("d_kT", [512, S], BF16), ("d_v", [S, 512], BF16),
                                ("d_mod", [128, 48], F32), ("d_yaT", [512, NOWN], BF16), ("d_x1", [NOWN, D], F32),
                                ("d_h2", [NOWN, D], BF16), ("d_qT", [8, 96, NOWN], BF16), ("d_logits", [128, 32 * 32], F32),
                                ("d_misc", [128, 2048], F32)):
            if name in self.debug:
                self.dbg[name] = self.dout(name, shape, dt)

        self.ps = [V(nc.alloc_psum_tensor("ps%d" % i, [128, 512], F32).ap(), self.newbuf("ps%d" % i)) for i in range(8)]
        with ExitStack() as top:
            self.top = top
            self.phase_A(top)
            self.P.barrier()
            if upto >= "B":
                self.phase_B()
            if upto >= "C":
                self.phase_C()
            if upto >= "D":
                self.phase_D()
            if upto >= "E":
                self.phase_E()
            if upto >= "F":
                self.phase_F()
            if upto >= "G":
                self.phase_G()
            if upto >= "H":
                self.phase_H(out)
            else:
                with ExitStack() as es:
                    z = self.sb(es, "zout", [128, D], F32)
                    self.memset(z, 0.0)
                    self.st(out[0:128, :], z)
                    self.P.barrier()
            self.debug_copies()
            self.P.barrier()
        return nc

    def debug_copies(self):
        mp = {"d_ob": "ob", "d_kT": "kT", "d_v": "v", "d_yaT": "yaT", "d_x1": "x1", "d_h2": "h2", "d_qT": "qT"}
        with ExitStack() as es:
            for dn, sn in mp.items():
                if dn not in self.dbg:
                    continue
                src = self.Sx[sn]
                dst = self.dbg[dn]
                if len(src.shape) == 3:
                    src = src.rearrange("a b c -> (a b) c")
                    dst = dst.rearrange("a b c -> (a b) c")
                rows, cols = src.shape
                t = self.sb(es, "dbg_" + sn, [128, cols], src.dtype)
                import os
                for r0 in range(0, min(rows, int(os.environ.get("DBGROWS", rows))), 128):
                    self.ld(t, src[r0:r0 + 128, :], q="pool")
                    self.st(dst[r0:r0 + 128, :], t)
            self.P.barrier()

    def phase_A(self, top):
        I = self.I
        cst = self.sb(top, "cst", [128, NCON], F32)
        self.ld(cst, I["consts"])
        self.cst = cst
        self.ident = cst[:, C_ID:C_ID + 128]
        self.triu = cst[:, C_TRIU:C_TRIU + 128]
        self.tris = cst[:, C_TRIS:C_TRIS + 128]
        self.masku = cst[:, C_MASKU:C_MASKU + 128]
        self.onesf = cst[:, C_ONES:C_ONES + 128]
        self.iotae = cst[:, C_IOTAE:C_IOTAE + 32]
        self.piota = cst[:, C_PIOTA:C_PIOTA + 8]
        self.blki = cst[:, C_BLKI:C_BLKI + 96]
        self.thr = cst[:, C_THR:C_THR + 16]
        self.identb = self.sb(top, "identb", [128, 128], BF16)
        self.cp(self.identb, self.ident)
        self.ustrb = self.sb(top, "ustrb", [128, 128], BF16)
        self.cp(self.ustrb, cst[:, C_USTR:C_USTR + 128])
        self.onesb = self.sb(top, "onesb", [128, 128], BF16)
        self.cp(self.onesb, self.onesf)
        self.modT = self.sb(top, "modT", [128, 48], F32)
        self.modbc = self.sb(top, "modbc", [128, 4, D], F32)
        self.A1T = self.sb(top, "A1T", [128, 8], F32)
        self.A2bc = self.sb(top, "A2bc", [128, D], F32)
        self.nfbc = self.sb(top, "nfbc", [128, D], F32)
        self.gnbc = self.sb(top, "gnbc", [128, 256], F32)
        self.c31 = self.sb(top, "c31", [128, 8], F32)
        self.gmt = self.sb(top, "gmt", [128, 3, 16, 32], F32)
        self.kmT = self.sb(top, "kmT", [128, 4, 32], BF16)
        self.kms = self.sb(top, "kms", [128, 4, 32], F32)
        self.ownidx = self.sb(top, "ownidx", [128, 32], I32)
        self.logits = self.sb(top, "logits", [128, 32, 32], F32)
        self.destI = self.sb(top, "destI", [128, 32, 4], I32)
        self.wk = self.sb(top, "wk", [128, 32, 4], F32)
        self.ld(self.nfbc, I["norm_final"].partition_broadcast(128))
        self.ld(self.gnbc, I["gla_norm"].partition_broadcast(128))
        self.ld(self.c31, I["rel31"].partition_broadcast(128))
        self.ld(self.gmt.re("p a b c -> p (a b c)"), I["gm"].partition_broadcast(128))
        self.ld(self.ownidx, I["own_idx"])
        with ExitStack() as es:
            cT = self.sb(es, "cT", [128, 8], F32)
            self.ld(cT, I["cvec"].rearrange("(c p) -> p c", p=128), slow=True)
            sT = self.sb(es, "sT", [128, 8], F32)
            self.act(sT, cT, AF.Silu)
            sTb = self.sb(es, "sTb", [128, 8, 128], F32)
            for c in range(8):
                self.ts(sTb[:, c, :], self.onesf, sT[:, c:c + 1], None, ALU.mult)
            badaT = self.sb(es, "badaT", [128, 48], F32)
            self.ld(badaT, I["b_ada"].rearrange("(c p) -> p c", p=128), slow=True)
            nmT = self.sb(es, "nmT", [128, 8], F32)
            self.ld(nmT, I["norm_mix"].rearrange("(c p) -> p c", p=128), slow=True)
            nffbc = self.sb(es, "nffbc", [128, D], F32)
            self.ld(nffbc, I["norm_ffn"].partition_broadcast(128))
            wa = [self.sb(es, "wa%d" % i, [128, 8, 512], F32) for i in range(2)]
            bb = [self.sb(es, "bb%d" % i, [128, 512], F32) for i in range(2)]
            psB = self.ps[7]
            save = self.ps
            self.ps = self.ps[0:7]
            self.psi = 0
            wsrc = I["w_ada"].rearrange("(c p) n -> p c n", p=128)
            for j in range(12):
                w = wa[j % 2]
                self.ld(w, wsrc[:, :, j * 512:(j + 1) * 512])
                if j >= 4:
                    b_ = bb[j % 2]
                    self.ld(b_, I["b_ada"][j * 512:(j + 1) * 512].partition_broadcast(128))
                    pa = self.nps()
                    for c in range(8):
                        self.mm(pa, sTb[:, c, :], w[:, c, :], st=(c == 0), sp=(c == 7))
                    k = (j - 4) // 2
                    self.tt(self.modbc[:, k, (j % 2) * 512:(j % 2) * 512 + 512], pa, b_, ALU.add)
                for g in range(4):
                    col = j * 4 + g
                    for c in range(8):
                        self.mm(psB[:, col:col + 1], w[:, c, g * 128:(g + 1) * 128], sT[:, c:c + 1], st=(c == 0), sp=(c == 7))
            self.tt(self.modT, psB[:, 0:48], badaT, ALU.add)
            self.ps = save
            self.psi = 0
            self.stt(self.A1T, self.modT[:, 8:16], 1.0, nmT, ALU.add, ALU.mult)
            self.stt(self.A2bc, self.modbc[:, 2, :], 1.0, nffbc, ALU.add, ALU.mult)
            if "d_mod" in self.dbg:
                self.st(self.dbg["d_mod"], self.modT)
            if "d_misc" in self.dbg:
                self.st(self.dbg["d_misc"][:, 0:1024], self.modbc[:, 0, :])
                self.st(self.dbg["d_misc"][:, 1024:2048], self.A2bc)
            self.P.barrier()

    def norm_to_hT(self, xt, hT_slice_fn, scr, AT, BT):
        junk, ss, xn = scr
        self.act(junk, xt, AF.Square, accum=ss)
        self.ts(ss, ss, 1.0 / D, 1e-6, ALU.mult, ALU.add)
        self.act(ss, ss, AF.Sqrt)
        self.P.op("dve", lambda e: e.reciprocal(out=ss.ap, in_=ss.ap), [ss.buf], [ss.buf])
        self.ts(xn, xt, ss[:, 0:1], None, ALU.mult)
        for half in range(2):
            pp = self.nps()
            for cc in range(4):
                c = half * 4 + cc
                self.tr(pp[:, cc * 128:(cc + 1) * 128], xn[:, c * 128:(c + 1) * 128], self.ident)
            for cc in range(4):
                c = half * 4 + cc
                self.act(hT_slice_fn(c), pp[:, cc * 128:(cc + 1) * 128], AF.Identity, bias=BT[:, c:c + 1], scale=AT[:, c:c + 1])

    def phase_B(self):
        I, Sx = self.I, self.Sx
        with ExitStack() as es:
            NCB = 3088
            wB = self.sb(es, "wB", [128, 8, NCB], BF16)
            wsrc = I["w_in"].rearrange("(c p) n -> p c n", p=128)
            import os
            SKIP = os.environ.get("SKIP", "")
            for c in range(0 if "wB" in SKIP else 8):
                self.ld(wB[:, c, :], wsrc[:, c, 512:3600], q="pool")
            wgk = self.sb(es, "wgk", [17, 512], F32)
            if "wgk" not in SKIP:
                self.ld(wgk[0:16, :], I["w_gk_up"])
                self.ld(wgk[16:17, :], I["b_gk"].rearrange("(o n) -> o n", o=1))
            xt = [self.sb(es, "xtB%d" % i, [128, D], F32) for i in range(2)]
            junk = self.sb(es, "junkB", [128, D], F32)
            ss = self.sb(es, "ssB", [128, 1], F32)
            xn = self.sb(es, "xnB", [128, D], F32)
            hT = self.sb(es, "hTB", [128, 8, 512], BF16)
            ktb = [self.sb(es, "ktb%d" % i, [128, 512], BF16) for i in range(2)]
            qbT = self.sb(es, "qbT", [128, 4, 512], F32)
            kbT = self.sb(es, "kbT", [128, 4, 512], F32)
            gkT = self.sb(es, "gkT", [17, 512], F32)
            if "msA" not in SKIP:
                self.memset(gkT, 1.0)
            vat = [self.sb(es, "vat%d" % i, [128, 512], BF16) for i in range(2)]
            kbt = [self.sb(es, "kbt%d" % i, [128, 512], F32) for i in range(4)]
            vbt = [self.sb(es, "vbt%d" % i, [128, D], BF16) for i in range(4)]
            ee = self.sb(es, "eeB", [128, 512], F32)
            ll = self.sb(es, "llB", [128, 512], F32)
            ebT = self.sb(es, "ebT", [128, 512], F32)
            enbT = self.sb(es, "enbT", [128, 512], F32)
            erem = self.sb(es, "erem", [128, 512], F32)
            qg = self.sb(es, "qg", [128, 4, 128], BF16)
            kg = self.sb(es, "kg", [128, 4, 128], BF16)
            kend = self.sb(es, "kend", [128, 512], BF16)
            attb = self.sb(es, "attb", [128, 4, 128], BF16)
            osb = [self.sb(es, "osb%d" % i, [128, D], F32) for i in range(2)]
            St = [self.sb(es, "St%d" % i, [128, 256], F32) for i in range(4)]
            Sb = [self.sb(es, "Sb%d" % i, [128, 256], BF16) for i in range(4)]
            for h in range(0 if "msB" in SKIP else 4):
                self.memset(St[h], 0.0)
                self.memset(Sb[h], 0.0)
            B1T = self.modT[:, 0:8]
            ntile = 0
            import os
            NG = int(os.environ.get("NG", S // 512))
            SKIP = os.environ.get("SKIP", "")
            for G in range(NG):
                t0 = G * 512
                for tt_ in range(4):
                    x_ = xt[ntile % 2]
                    ntile += 1
                    self.ld(x_, I["xf"][t0 + tt_ * 128:t0 + (tt_ + 1) * 128, :], q="pool")
                    if "norm" not in SKIP:
                        self.norm_to_hT(x_, lambda c, tt_=tt_: hT[:, c, tt_ * 128:(tt_ + 1) * 128], (junk, ss, xn), self.A1T, B1T)
                for gi in range(0 if "ka" in SKIP else 4):
                    pp = self.nps()
                    for c in range(8):
                        self.mm(pp, wB[:, c, gi * 128:(gi + 1) * 128], hT[:, c, :], st=(c == 0), sp=(c == 7))
                    kt_ = ktb[gi % 2]
                    for hb_ in range(2):
                        self.act(kt_[:, hb_ * 256:(hb_ + 1) * 256], pp[:, hb_ * 256:(hb_ + 1) * 256], AF.Identity,
                                 accum=self.kms[:, gi, 2 * G + hb_:2 * G + hb_ + 1])
                    if "stk" not in SKIP:
                        self.st(Sx["kT"][gi * 128:(gi + 1) * 128, t0:t0 + 512], kt_)
                for gi in range(0 if "qbkb" in SKIP else 4):
                    pp = self.nps()
                    for c in range(8):
                        self.mm(pp, wB[:, c, 1024 + gi * 128:1024 + (gi + 1) * 128], hT[:, c, :], st=(c == 0), sp=(c == 7))
                    self.cp(qbT[:, gi, :], pp, eng="act")
                    pp = self.nps()
                    for c in range(8):
                        self.mm(pp, wB[:, c, 1536 + gi * 128:1536 + (gi + 1) * 128], hT[:, c, :], st=(c == 0), sp=(c == 7))
                    self.cp(kbT[:, gi, :], pp)
                if "gk" not in SKIP:
                    pp = self.nps()
                    for c in range(8):
                        self.mm(pp[0:16, :], wB[:, c, 3072:3088], hT[:, c, :], st=(c == 0), sp=(c == 7))
                    self.cp(gkT[0:16, :], pp[0:16, :])
                for tt_ in range(0 if "tm" in SKIP else 4):
                    hs = lambda c: hT[:, c, tt_ * 128:(tt_ + 1) * 128]
                    pp = self.nps()
                    for c in range(8):
                        self.mm(pp, hs(c), wB[:, c, 512:1024], st=(c == 0), sp=(c == 7))
                    va_ = vat[tt_ % 2]
                    self.cp(va_, pp, eng="act")
                    self.st(Sx["v"][t0 + tt_ * 128:t0 + (tt_ + 1) * 128, :], va_)
                    pp = self.nps()
                    for c in range(8):
                        self.mm(pp, hs(c), wB[:, c, 1536:2048], st=(c == 0), sp=(c == 7))
                    self.cp(kbt[tt_], pp)
                    for hf in range(2):
                        pp = self.nps()
                        for c in range(8):
                            self.mm(pp, hs(c), wB[:, c, 2048 + hf * 512:2048 + (hf + 1) * 512], st=(c == 0), sp=(c == 7))
                        self.cp(vbt[tt_][:, hf * 512:(hf + 1) * 512], pp, eng="act")
                for tt_ in range(0 if "gla" in SKIP else 4):
                    sl = slice(tt_ * 128, (tt_ + 1) * 128)
                    pz = self.nps()
                    self.mm(pz, gkT[0:17, sl], wgk[0:17, :])
                    self.act(ee, pz, AF.Exp, scale=-1.0)
                    self.act(ll, ee, AF.Ln, bias=1.0)
                    pr = self.nps()
                    self.mm(pr, self.tris, ll)
                    pbt = self.nps()
                    for h in range(4):
                        self.mm(pbt[:, h * 128:(h + 1) * 128], ll[:, h * 128:(h + 1) * 128], self.triu)
                    self.act(ebT, pbt, AF.Exp)
                    self.act(enbT, pbt, AF.Exp, scale=-1.0)
                    self.act(erem, pr, AF.Exp)
                    self.stt(qg, qbT[:, :, sl], 128.0 ** -0.5, ebT.re("p (h c) -> p h c", h=4), ALU.mult, ALU.mult)
                    self.tt(kg, kbT[:, :, sl], enbT.re("p (h c) -> p h c", h=4), ALU.mult)
                    self.tt(kend, kbt[tt_], erem, ALU.mult)
                    pa = self.nps()
                    for h in range(4):
                        self.mm(pa[:, h * 128:(h + 1) * 128], kg[:, h, :], qg[:, h, :])
                    for h in range(4):
                        self.tt(attb[:, h, :], pa[:, h * 128:(h + 1) * 128], self.masku, ALU.mult)
                    o_ = osb[tt_ % 2]
                    for hp in range(2):
                        po = self.nps()
                        for hh in range(2):
                            h = hp * 2 + hh
                            self.mm(po[:, hh * 256:(hh + 1) * 256], attb[:, h, :], vbt[tt_][:, h * 256:(h + 1) * 256], st=True, sp=False)
                            self.mm(po[:, hh * 256:(hh + 1) * 256], qg[:, h, :], Sb[h], st=False, sp=True)
                        self.cp(o_[:, hp * 512:(hp + 1) * 512], po, eng=("act" if hp else "dve"))
                    self.st(Sx["ob"][t0 + tt_ * 128:t0 + (tt_ + 1) * 128, :], o_)
                    for hp in range(2):
                        pu = self.nps()
                        for hh in range(2):
                            h = hp * 2 + hh
                            self.mm(pu[:, hh * 256:(hh + 1) * 256], kend[:, h * 128:(h + 1) * 128], vbt[tt_][:, h * 256:(h + 1) * 256])
                        for hh in range(2):
                            h = hp * 2 + hh
                            self.stt(St[h], St[h], ebT[:, h * 128 + 127:h * 128 + 128], pu[:, hh * 256:(hh + 1) * 256], ALU.mult, ALU.add)
                            self.cp(Sb[h], St[h], eng="act")
            self.ts(self.kmT, self.kms, 1.0 / 256.0, None, ALU.mult)
            self.P.barrier()

    def phase_C(self):
        I, Sx = self.I, self.Sx
        with ExitStack() as es:
            wq = self.sb(es, "wq", [128, 8, 512], BF16)
            wsrc = I["w_in"].rearrange("(c p) n -> p c n", p=128)
            for c in range(0, 8, 2):
                self.ld(wq[:, c:c + 2, :], wsrc[:, c:c + 2, 0:512], q="pool")
            xt = [self.sb(es, "xtC%d" % i, [128, D], F32) for i in range(2)]
            junk = self.sb(es, "junkC", [128, D], F32)
            ss = self.sb(es, "ssC", [128, 1], F32)
            xn = self.sb(es, "xnC", [128, D], F32)
            hT = self.sb(es, "hTC", [128, 8, 512], BF16)
            qp = [self.sb(es, "qp%d" % i, [128, 512], BF16) for i in range(2)]
            mrow = [self.sb(es, "mrow%d" % i, [96, 512], BF16) for i in range(2)]
            mpad = [self.sb(es, "mpad%d" % i, [128, 96], F32) for i in range(2)]
            for m_ in mpad:
                self.memset(m_, 0.0)
            gmk = self.sb(es, "gmk", [128, 32], F32)
            m8 = self.sb(es, "m8", [128, 8], F32)
            sel = self.sb(es, "sel", [128, 32], F32)
            B1T = self.modT[:, 0:8]
            ntile = 0
            nm = 0
            for G in range(NOWN // 512):
                t0 = G * 512
                for tt_ in range(4):
                    x_ = xt[ntile % 2]
                    ntile += 1
                    self.ld(x_, I["xo"][t0 + tt_ * 128:t0 + (tt_ + 1) * 128, :], q="pool")
                    self.norm_to_hT(x_, lambda c, tt_=tt_: hT[:, c, tt_ * 128:(tt_ + 1) * 128], (junk, ss, xn), self.A1T, B1T)
                for gi in range(4):
                    pp = self.nps()
                    for c in range(8):
                        self.mm(pp, wq[:, c, gi * 128:(gi + 1) * 128], hT[:, c, :], st=(c == 0), sp=(c == 7))
                    q_ = qp[gi % 2]
                    self.act(q_, pp, AF.Copy, scale=0.125)
                    for hh in range(2):
                        h = gi * 2 + hh
                        hb = hh * 64
                        self.st(Sx["qT"][h, 0:64, t0:t0 + 512], q_[hb:hb + 64, :])
                        mr = mrow[nm % 2]
                        nm += 1
                        for tt_ in range(4):
                            i = 2 * G + tt_ // 2
                            pg = self.nps()
                            self.mm(pg[:, 0:32], q_[hb:hb + 64, tt_ * 128:(tt_ + 1) * 128], self.kmT[hb:hb + 64, gi, :])
                            self.tt(gmk, pg[:, 0:32], self.gmt[:, 0, i, :], ALU.add)
                            self.P.op("dve", lambda e: e.max(out=m8.ap, in_=gmk.ap), [gmk.buf], [m8.buf])
                            self.ts(sel, gmk, m8[:, 2:3], None, ALU.is_ge)
                            self.tt(sel, sel, self.gmt[:, 1, i, :], ALU.mult)
                            self.tt(sel, sel, self.gmt[:, 2, i, :], ALU.add)
                            mp_ = mpad[tt_ % 2]
                            self.ts(mp_[:, 64:96], sel, -1.0, -NEG, ALU.add, ALU.mult)
                            pt = self.nps()
                            self.tr(pt[0:96, 0:128], mp_, self.ident)
                            self.cp(mr[64:96, tt_ * 128:(tt_ + 1) * 128], pt[64:96, 0:128], eng="act")
                        self.st(Sx["qT"][h, 64:96, t0:t0 + 512], mr[64:96, :])
            self.P.barrier()

    def phase_D(self):
        I, Sx = self.I, self.Sx
        with ExitStack() as es:
            kaug = [self.sb(es, "kaug%d" % i, [96, S], BF16) for i in range(2)]
            vaug = [self.sb(es, "vaug%d" % i, [128, 64, 65], BF16) for i in range(2)]
            qaug = [self.sb(es, "qaug%d" % i, [96, NOWN], BF16) for i in range(2)]
            bt = [self.sb(es, "bt%d" % i, [128, 1536], F32) for i in range(2)]
            sbias = [self.sb(es, "sbias%d" % i, [128, 512], F32) for i in range(2)]
            pT = [self.sb(es, "pT%d" % i, [128, 512], BF16) for i in range(3)]
            osb = [self.sb(es, "osbD%d" % i, [65, 256], F32) for i in range(2)]
            rinv = [self.sb(es, "rinv%d" % i, [65, 256], F32) for i in range(2)]
            yo = [self.sb(es, "yo%d" % i, [64, 256], BF16) for i in range(2)]
            for i in range(2):
                self.ld(kaug[i][64:96, :], I["koh"], q="pool")
                self.memset(vaug[i], 1.0)
            vsrc = Sx["v"].rearrange("(t p) f -> p t f", p=128)
            npT = 0
            nsb = 0
            nfin = 0
            psos = [self.ps[6], self.ps[7]]
            save = self.ps
            self.ps = self.ps[0:6]
            self.psi = 0
            state = {"npT": 0, "nsb": 0, "nfin": 0}

            def emit_S(ka, qa, b_, i, j):
                qs = qa[0:96, i * 256:(i + 1) * 256]
                pss = self.nps()
                for kt in range(2):
                    self.mm(pss[:, kt * 256:(kt + 1) * 256], ka[0:96, (2 * j + kt) * 128:(2 * j + kt + 1) * 128], qs)
                src = pss
                if j >= 2 * i - 1:
                    which = j - (2 * i - 1)
                    sb_ = sbias[state["nsb"] % 2]
                    state["nsb"] += 1
                    self.tt(sb_, pss, b_[:, which * 512:(which + 1) * 512], ALU.add)
                    src = sb_
                p_ = pT[state["npT"] % 3]
                state["npT"] += 1
                self.act(p_, src, AF.Exp)
                return p_

            def emit_PV(va, h, i, j, p_):
                nj = 2 * i + 2
                pso = psos[i % 2]
                for kt in range(2):
                    self.mm(pso[0:65, 0:256], va[:, 2 * j + kt, :], p_[:, kt * 256:(kt + 1) * 256],
                            st=(j == 0 and kt == 0), sp=(j == nj - 1 and kt == 1))
                if j == nj - 1:
                    nf = state["nfin"]
                    o_, r_, y_ = osb[nf % 2], rinv[nf % 2], yo[nf % 2]
                    state["nfin"] += 1
                    self.cp(o_, pso[0:65, 0:256])
                    self.P.op("dve", lambda e: e.reciprocal(out=r_[64:65, :].ap, in_=o_[64:65, :].ap), [o_.buf], [r_.buf])
                    pb = self.nps()
                    self.mm(pb[0:64, 0:256], self.onesf[64:65, 0:64], r_[64:65, :])
                    self.tt(y_, pb[0:64, 0:256], o_[0:64, :], ALU.mult)
                    self.st(Sx["yaT"][h * 64:(h + 1) * 64, i * 256:(i + 1) * 256], y_)

            for h in range(8):
                ka, va, qa, b_ = kaug[h % 2], vaug[h % 2], qaug[h % 2], bt[h % 2]
                self.ld(ka[0:64, :], Sx["kT"][h * 64:(h + 1) * 64, :], q="pool")
                for t8 in range(8):
                    self.ld(va[:, t8 * 8:(t8 + 1) * 8, 0:64], vsrc[:, t8 * 8:(t8 + 1) * 8, h * 64:(h + 1) * 64], q="pool")
                self.ld(qa, Sx["qT"][h], q="pool")
                self.ld(b_, I["btab"][h])
                self.ts(b_, b_, self.c31[:, h:h + 1], None, ALU.subtract)
                items = [(i, j) for i in range(16) for j in range(2 * i + 2)]
                prev = None
                for (i, j) in items:
                    p_ = emit_S(ka, qa, b_, i, j)
                    if prev is not None:
                        emit_PV(va, h, *prev)
                    prev = (i, j, p_)
                emit_PV(va, h, *prev)
            self.ps = save
            self.psi = 0
            self.P.barrier()

    def recip(self, t):
        self.P.op("dve", lambda e: e.reciprocal(out=t.ap, in_=t.ap), [t.buf], [t.buf])

    def phase_E(self):
        I, Sx = self.I, self.Sx
        GT = 256
        with ExitStack() as es:
            wsrc = I["w_in"].rearrange("(c p) n -> p c n", p=128)
            wE = self.sb(es, "wE", [128, 8, 3072], BF16)
            for c in range(8):
                self.ld(wE[:, c, :], wsrc[:, c, 3600:6672], q="pool")
            wpa = self.sb(es, "wpa", [64, 8, D], BF16)
            wpas = I["w_pa"].rearrange("(h d) n -> d h n", d=64)
            for h in range(0, 8, 2):
                self.ld(wpa[:, h:h + 2, :], wpas[:, h:h + 2, :], q="pool")
            wpb = self.sb(es, "wpb", [128, 8, D], BF16)
            wpbs = I["w_pb"].rearrange("(c p) n -> p c n", p=128)
            wout = self.sb(es, "wout", [128, 8, D], BF16)
            wouts = I["w_out"].rearrange("(c p) n -> p c n", p=128)
            for c in range(0, 8, 2):
                self.ld(wpb[:, c:c + 2, :], wpbs[:, c:c + 2, :], q="pool")
                self.ld(wout[:, c:c + 2, :], wouts[:, c:c + 2, :], q="pool")
            wr = self.sb(es, "wr", [128, 8, 32], F32)
            self.ld(wr, I["w_router"].rearrange("(c p) n -> p c n", p=128))
            br = self.sb(es, "br", [1, 32], F32)
            self.ld(br, I["b_router"].rearrange("(o n) -> o n", o=1))
            xg = self.sb(es, "xg", [128, 2, D], F32)
            A_ = self.sb(es, "EA", [128, D], F32)
            B_ = self.sb(es, "EB", [128, D], F32)
            C_ = self.sb(es, "EC", [128, D], F32)
            D_ = self.sb(es, "ED", [128, D], F32)
            ss = self.sb(es, "ssE", [128, 1], F32)
            ss4 = self.sb(es, "ss4", [128, 4], F32)
            hT = self.sb(es, "hTE", [128, 8, GT], BF16)
            yaT = self.sb(es, "yaTE", [64, 8, GT], BF16)
            ybT = self.sb(es, "ybTE", [128, 8, GT], BF16)
            mixT = self.sb(es, "mixT", [128, 8, GT], BF16)
            sgA = self.sb(es, "sgA", [128, GT], F32)
            sgB = self.sb(es, "sgB", [128, GT], F32)
            t1 = self.sb(es, "t1E", [128, GT], F32)
            t2 = self.sb(es, "t2E", [128, GT], F32)
            h2b = self.sb(es, "h2b", [128, D], BF16)
            B1T = self.modT[:, 0:8]
            yas = Sx["yaT"].rearrange("(h d) t -> d h t", d=64)
            for G in range(NOWN // GT):
                t0 = G * GT
                for tt_ in range(2):
                    self.ld(xg[:, tt_, :], I["xo"][t0 + tt_ * 128:t0 + (tt_ + 1) * 128, :], q="pool")
                    self.norm_to_hT(xg[:, tt_, :], lambda c, tt_=tt_: hT[:, c, tt_ * 128:(tt_ + 1) * 128], (A_, ss, B_), self.A1T, B1T)
                self.ld(yaT, yas[:, :, t0:t0 + GT], q="pool")
                for tt_ in range(2):
                    t = G * 2 + tt_
                    sl = slice(tt_ * 128, (tt_ + 1) * 128)
                    self.gather(A_, Sx["ob"], self.ownidx[:, t:t + 1])
                    for hf in range(2):
                        pp = self.nps()
                        for c in range(8):
                            self.mm(pp, hT[:, c, sl], wE[:, c, hf * 512:(hf + 1) * 512], st=(c == 0), sp=(c == 7))
                        self.act(B_[:, hf * 512:(hf + 1) * 512], pp, AF.Silu)
                    for h in range(4):
                        self.act(D_[:, h * 256:(h + 1) * 256], A_[:, h * 256:(h + 1) * 256], AF.Square, accum=ss4[:, h:h + 1])
                    self.ts(ss4, ss4, 1.0 / 256.0, 1e-6, ALU.mult, ALU.add)
                    self.act(ss4, ss4, AF.Sqrt)
                    self.recip(ss4)
                    for h in range(4):
                        self.stt(C_[:, h * 256:(h + 1) * 256], A_[:, h * 256:(h + 1) * 256], ss4[:, h:h + 1], self.gnbc, ALU.mult, ALU.mult)
                    self.tt(C_, C_, B_, ALU.mult)
                    for half in range(2):
                        pp = self.nps()
                        for cc in range(4):
                            c = half * 4 + cc
                            self.tr(pp[:, cc * 128:(cc + 1) * 128], C_[:, c * 128:(c + 1) * 128], self.ident)
                        self.cp(ybT[:, half * 4:(half + 1) * 4, sl], pp.re("p (a b) -> p a b", a=4), eng=("act" if half else "dve"))
                for dg in range(8):
                    pga = self.nps()
                    for c in range(8):
                        self.mm(pga[:, 0:GT], wE[:, c, 1024 + dg * 128:1024 + (dg + 1) * 128], hT[:, c, :], st=(c == 0), sp=(c == 7))
                    self.act(sgA, pga[:, 0:GT], AF.Sigmoid)
                    pgb = self.nps()
                    for c in range(8):
                        self.mm(pgb[:, 0:GT], wE[:, c, 2048 + dg * 128:2048 + (dg + 1) * 128], hT[:, c, :], st=(c == 0), sp=(c == 7))
                    self.act(sgB, pgb[:, 0:GT], AF.Sigmoid)
                    ppa = self.nps()
                    for h in range(8):
                        self.mm(ppa[:, 0:GT], wpa[0:64, h, dg * 128:(dg + 1) * 128], yaT[0:64, h, :], st=(h == 0), sp=(h == 7))
                    ppb = self.nps()
                    for c in range(8):
                        self.mm(ppb[:, 0:GT], wpb[:, c, dg * 128:(dg + 1) * 128], ybT[:, c, :], st=(c == 0), sp=(c == 7))
                    self.tt(t1, ppa[:, 0:GT], sgA, ALU.mult)
                    self.tt(t2, ppb[:, 0:GT], sgB, ALU.mult)
                    self.tt(mixT[:, dg, :], t1, t2, ALU.add)
                for tt_ in range(2):
                    t = G * 2 + tt_
                    sl = slice(tt_ * 128, (tt_ + 1) * 128)
                    rows = slice(t0 + tt_ * 128, t0 + (tt_ + 1) * 128)
                    for hf in range(2):
                        pp = self.nps()
                        for c in range(8):
                            self.mm(pp, mixT[:, c, sl], wout[:, c, hf * 512:(hf + 1) * 512], st=(c == 0), sp=(c == 7))
                        self.tt(A_[:, hf * 512:(hf + 1) * 512], pp, self.modbc[:, 0, hf * 512:(hf + 1) * 512], ALU.mult)
                    self.tt(A_, A_, xg[:, tt_, :], ALU.add)
                    self.st(Sx["x1"][rows, :], A_)
                    self.act(B_, A_, AF.Square, accum=ss)
                    self.ts(ss, ss, 1.0 / D, 1e-6, ALU.mult, ALU.add)
                    self.act(ss, ss, AF.Sqrt)
                    self.recip(ss)
                    self.ts(B_, A_, ss[:, 0:1], None, ALU.mult)
                    self.tt(C_, B_, self.A2bc, ALU.mult)
                    self.tt(C_, C_, self.modbc[:, 1, :], ALU.add)
                    self.cp(h2b, C_, eng="act")
                    self.st(Sx["h2"][rows, :], h2b)
                    for half in range(2):
                        pp = self.nps()
                        for cc in range(4):
                            c = half * 4 + cc
                            self.tr(pp[:, cc * 128:(cc + 1) * 128], C_[:, c * 128:(c + 1) * 128], self.ident)
                        self.cp(D_[:, half * 512:(half + 1) * 512], pp, eng=("act" if half else "dve"))
                    pl = self.nps()
                    for c in range(8):
                        self.mm(pl[:, 0:32], D_[:, c * 128:(c + 1) * 128], wr[:, c, :], st=(c == 0), sp=False)
                    self.mm(pl[:, 0:32], self.onesf[0:1, 0:128], br[0:1, :], st=False, sp=True)
                    self.cp(self.logits[:, t, :], pl[:, 0:32])
            if "d_logits" in self.dbg:
                self.st(self.dbg["d_logits"], self.logits.re("p a b -> p (a b)"))
            self.P.barrier()

    def phase_F(self):
        I, Sx = self.I, self.Sx
        top = self.top
        self.widx = self.sb(top, "widx", [128, NBLK], I32)
        self.bgT = self.sb(top, "bgT", [128, 16, NBLK], F32)
        self.ohT = self.sb(top, "ohT", [32, NBLK], F32)
        self.bdh = self.sb(top, "bdh", [32, D], BF16)
        self.bdl = self.sb(top, "bdl", [32, D], BF16)
        with ExitStack() as es:
            M = self.sb(es, "Moh", [128, 32, 32], BF16)
            m8 = self.sb(es, "m8F", [128, 8], F32)
            idx8 = self.sb(es, "idx8", [128, 8], U32)
            idxall = self.sb(es, "idxall", [128, 32, 4], F32)
            e4 = self.sb(es, "e4", [128, 4], F32)
            s1 = self.sb(es, "s1F", [128, 1], F32)
            negm = self.sb(es, "negm", [128, 1], F32)
            rank = self.sb(es, "rank", [128, 32, 32], F32)
            base = self.sb(es, "base", [128, 32], F32)
            nblk = self.sb(es, "nblk", [128, 32], F32)
            ps_ = self.sb(es, "pstart", [128, 33], F32)
            oh = self.sb(es, "ohF", [128, 32], F32)
            junk = self.sb(es, "junkF", [128, 32], F32)
            destf = self.sb(es, "destf", [128, 32, 4], F32)
            acc = self.sb(es, "accF", [128, NBLK], F32)
            be = self.sb(es, "beF", [128, NBLK], F32)
            widxf = self.sb(es, "widxf", [128, NBLK], F32)
            bgu = self.sb(es, "bguF", [32, 2048], F32)
            bd = self.sb(es, "bdF", [32, D], F32)
            bd2 = self.sb(es, "bd2F", [32, D], F32)
            h2t = [self.sb(es, "h2t%d" % i, [128, D], BF16) for i in range(2)]
            self.ld(bgu, I["bgu"])
            self.ld(bd, I["b_down"])
            for t in range(32):
                lg = self.logits[:, t, :]
                self.P.op("dve", lambda e: e.max(out=m8.ap, in_=lg.ap), [lg.buf], [m8.buf])
                self.P.op("dve", lambda e: e.max_index(out=idx8.ap, in_max=m8.ap, in_values=lg.ap), [m8.buf, lg.buf], [idx8.buf])
                self.cp(idxall[:, t, :], idx8[:, 0:4])
                self.ts(M[:, t, :], lg, m8[:, 3:4], None, ALU.is_ge)
                self.ts(negm, m8[:, 0:1], -1.0, None, ALU.mult)
                self.act(e4, m8[:, 0:4], AF.Exp, bias=negm[:, 0:1], accum=s1)
                self.recip(s1)
                self.ts(self.wk[:, t, :], e4, s1[:, 0:1], None, ALU.mult)
            self.memset(base, 0.0)
            for t in range(32):
                pr = self.nps()
                self.mm(pr[:, 0:32], self.ustrb, M[:, t, :])
                self.mm(pr[:, 32:64], self.onesb, M[:, t, :])
                self.tt(rank[:, t, :], pr[:, 0:32], base, ALU.add)
                self.tt(base, base, pr[:, 32:64], ALU.add)
            self.memset(nblk, 0.0)
            for m in range(16):
                self.stt(nblk, base, self.thr[:, m:m + 1], nblk, ALU.is_gt, ALU.add)
            self.memset(ps_, 0.0)
            for e_ in range(1, 33):
                self.tt(ps_[:, e_:e_ + 1], ps_[:, e_ - 1:e_], nblk[:, e_ - 1:e_], ALU.add)
            for t in range(32):
                self.stt(rank[:, t, :], ps_[:, 0:32], 256.0, rank[:, t, :], ALU.mult, ALU.add)
            for t in range(32):
                for k in range(4):
                    self.ts(oh, self.iotae, idxall[:, t, k:k + 1], None, ALU.is_equal)
                    self.tt(oh, oh, rank[:, t, :], ALU.mult)
                    self.act(junk, oh, AF.Identity, accum=destf[:, t, k:k + 1])
            self.cp(self.destI, destf)
            self.memset(acc, 0.0)
            for e_ in range(32):
                self.stt(acc, self.blki, ps_[:, e_ + 1:e_ + 2], acc, ALU.is_ge, ALU.add)
            self.ts(be, acc, 31.0, None, ALU.min)
            self.ts(widxf, be, 128.0, self.piota[:, 0:1], ALU.mult, ALU.add)
            self.cp(self.widx, widxf)
            self.ts(self.ohT, be[0:32, :], self.piota[0:32, 0:1], None, ALU.is_equal)
            for ft in range(16):
                pp = self.nps()
                self.mm(pp[:, 0:NBLK], bgu[0:32, ft * 128:(ft + 1) * 128], self.ohT[0:32, :])
                self.cp(self.bgT[:, ft, :], pp[:, 0:NBLK])
            self.cp(self.bdh, bd)
            self.cp(bd2, self.bdh)
            self.tt(self.bdl, bd, bd2, ALU.subtract)
            for t in range(32):
                h_ = h2t[t % 2]
                self.ld(h_, Sx["h2"][t * 128:(t + 1) * 128, :], q="pool")
                for k in range(4):
                    self.scatter(Sx["xpad"], h_, self.destI[:, t, k:k + 1])
            if "d_misc" in self.dbg:
                self.st(self.dbg["d_misc"][:, 0:NBLK], be)
                self.st(self.dbg["d_misc"][:, 128:256], destf.re("p a b -> p (a b)"))
                self.st(self.dbg["d_misc"][:, 256:384], self.wk.re("p a b -> p (a b)"))
                self.st(self.dbg["d_misc"][:, 384:416], base)
            self.P.barrier()

    def phase_G(self):
        I, Sx = self.I, self.Sx
        import os
        NB = int(os.environ.get("NBG", NBLK))
        with ExitStack() as es:
            xb = [self.sb(es, "xb%d" % i, [128, 2, D], BF16) for i in range(2)]
            xT = self.sb(es, "xTG", [128, 8, 256], BF16)
            wg = [self.sb(es, "wg%d" % i, [128, 8, D], BF16) for i in range(2)]
            wu = [self.sb(es, "wu%d" % i, [128, 8, D], BF16) for i in range(2)]
            wd = [self.sb(es, "wd%d" % i, [128, 8, D], BF16) for i in range(2)]
            gm = [self.sb(es, "gmG%d" % i, [128, 256], F32) for i in range(2)]
            sg = [self.sb(es, "sgG%d" % i, [128, 256], F32) for i in range(2)]
            u1 = [self.sb(es, "u1G%d" % i, [128, 256], F32) for i in range(2)]
            actT = self.sb(es, "actT", [128, 8, 256], BF16)
            ohb = [self.sb(es, "ohb%d" % i, [32, 128], BF16) for i in range(2)]
            yb = [self.sb(es, "ybG%d" % i, [128, 2, D], F32) for i in range(2)]
            xsrc = Sx["xpad"].rearrange("(b a p) n -> b p a n", a=2, p=128)
            ydst = Sx["ypad"].rearrange("(b a p) n -> b p a n", a=2, p=128)
            n2 = 0
            for blk in range(NB):
                x_ = xb[blk % 2]
                wg_, wu_, wd_ = wg[blk % 2], wu[blk % 2], wd[blk % 2]
                self.ld(x_, xsrc[blk], q="pool")
                self.gather(wg_.re("p c n -> p (c n)"), I["w_gate"], self.widx[:, blk:blk + 1])
                self.gather(wu_.re("p c n -> p (c n)"), I["w_up"], self.widx[:, blk:blk + 1])
                self.gather(wd_.re("p c n -> p (c n)"), I["w_down"], self.widx[:, blk:blk + 1])
                for a in range(2):
                    for half in range(2):
                        pp = self.nps()
                        ppb = V(pp.ap.bitcast(BF16), pp.buf)
                        for cc in range(4):
                            c = half * 4 + cc
                            self.tr(ppb[:, cc * 128:(cc + 1) * 128], x_[:, a, c * 128:(c + 1) * 128], self.identb)
                        self.cp(xT[:, half * 4:(half + 1) * 4, a * 128:(a + 1) * 128], ppb[:, 0:512].re("p (a b) -> p a b", a=4),
                                eng=("act" if half else "dve"))
                oh_ = ohb[blk % 2]
                self.ts(oh_, self.onesf[0:32, 0:128], self.ohT[0:32, blk:blk + 1], None, ALU.mult)
                for ft in range(8):
                    g_, s_, u_ = gm[n2 % 2], sg[n2 % 2], u1[n2 % 2]
                    n2 += 1
                    pg = self.nps()
                    for c in range(8):
                        self.mm(pg[:, 0:256], wg_[:, c, ft * 128:(ft + 1) * 128], xT[:, c, :], st=(c == 0), sp=(c == 7))
                    pu = self.nps()
                    for c in range(8):
                        self.mm(pu[:, 0:256], wu_[:, c, ft * 128:(ft + 1) * 128], xT[:, c, :], st=(c == 0), sp=(c == 7))
                    self.ts(g_, pg[:, 0:256], self.bgT[:, ft, blk:blk + 1], 7.0, ALU.add, ALU.min)
                    self.act(s_, g_, AF.Sigmoid, scale=1.702)
                    self.ts(u_, pu[:, 0:256], self.bgT[:, 8 + ft, blk:blk + 1], 7.0, ALU.add, ALU.min)
                    self.ts(u_, u_, -7.0, 1.0, ALU.max, ALU.add)
                    self.tt(g_, g_, s_, ALU.mult)
                    self.tt(actT[:, ft, :], g_, u_, ALU.mult)
                y_ = yb[blk % 2]
                for a in range(2):
                    for dh in range(2):
                        py = self.nps()
                        for ft in range(8):
                            self.mm(py, actT[:, ft, a * 128:(a + 1) * 128], wd_[:, ft, dh * 512:(dh + 1) * 512], st=(ft == 0), sp=False)
                        self.mm(py, oh_[0:32, :], self.bdh[0:32, dh * 512:(dh + 1) * 512], st=False, sp=False)
                        self.mm(py, oh_[0:32, :], self.bdl[0:32, dh * 512:(dh + 1) * 512], st=False, sp=True)
                        self.cp(y_[:, a, dh * 512:(dh + 1) * 512], py, eng=("act" if dh else "dve"))
                self.st(ydst[blk], y_)
            self.P.barrier()

    def phase_H(self, out):
        I, Sx = self.I, self.Sx
        with ExitStack() as es:
            x1t = [self.sb(es, "x1t%d" % i, [128, D], F32) for i in range(2)]
            yk = [self.sb(es, "yk%d" % i, [128, D], F32) for i in range(8)]
            acc = [self.sb(es, "accH%d" % i, [128, D], F32) for i in range(2)]
            junk = self.sb(es, "junkH", [128, D], F32)
            ss = self.sb(es, "ssH", [128, 1], F32)
            o_ = [self.sb(es, "oH%d" % i, [128, D], F32) for i in range(2)]
            for t in range(32):
                rows = slice(t * 128, (t + 1) * 128)
                x_ = x1t[t % 2]
                a_ = acc[t % 2]
                self.ld(x_, Sx["x1"][rows, :], q="pool")
                ys = [yk[(t % 2) * 4 + k] for k in range(4)]
                for k in range(4):
                    self.gather(ys[k], Sx["ypad"], self.destI[:, t, k:k + 1])
                self.ts(a_, ys[0], self.wk[:, t, 0:1], None, ALU.mult)
                for k in range(1, 4):
                    self.stt(a_, ys[k], self.wk[:, t, k:k + 1], a_, ALU.mult, ALU.add)
                self.tt(a_, a_, self.modbc[:, 3, :], ALU.mult)
                self.tt(a_, a_, x_, ALU.add)
                self.act(junk, a_, AF.Square, accum=ss)
                self.ts(ss, ss, 1.0 / D, 1e-6, ALU.mult, ALU.add)
                self.act(ss, ss, AF.Sqrt)
                self.recip(ss)
                oo = o_[t % 2]
                self.stt(oo, a_, ss[:, 0:1], self.nfbc, ALU.mult, ALU.mult)
                self.st(out[rows, :], oo)
            self.P.barrier()


def _bucket(n):
    n = np.maximum(n, 0)
    me = 16
    nf = np.maximum(n, me).astype(np.float32)
    large = me + (np.log(nf / me) / math.log(128 / me) * (32 - me)).astype(np.int32)
    large = np.minimum(large, 31)
    return np.where(n < me, n, large)


def make_consts():
    c = np.zeros((128, NCON), np.float32)
    p = np.arange(128)[:, None]
    f = np.arange(128)[None, :]
    c[:, C_ID:C_ID + 128] = (p == f)
    c[:, C_TRIU:C_TRIU + 128] = np.where(p <= f, -1.0 / 16.0, 0.0)
    c[:, C_TRIS:C_TRIS + 128] = np.where(p > f, -1.0 / 16.0, 0.0)
    c[:, C_MASKU:C_MASKU + 128] = (p <= f)
    c[:, C_USTR:C_USTR + 128] = (p < f)
    c[:, C_ONES:C_ONES + 128] = 1.0
    c[:, C_IOTAE:C_IOTAE + 32] = np.arange(32)[None, :]
    c[:, C_PIOTA:C_PIOTA + 8] = p + 128 * np.arange(8)[None, :]
    c[:, C_BLKI:C_BLKI + 96] = np.arange(96)[None, :]
    c[:, C_THR:C_THR + 16] = 256.0 * np.arange(16)[None, :]
    return c


def make_core_tables(p, rel_bias):
    gm = np.zeros((3, 16, 32), np.float32)
    for i in range(16):
        g = 2 * i + p
        j = np.arange(32)
        gm[0, i] = np.where(j < g, 0.0, -1e30)
        gm[1, i] = (j < g)
        gm[2, i] = (j == g)
    k = np.arange(256)[:, None]
    q = np.arange(256)[None, :]
    idx = np.zeros((3, 256, 256), np.int64)
    for which in range(3):
        delta = p + 1 - which
        if delta >= 2:
            idx[which] = 31
        elif delta == 1:
            idx[which] = _bucket(256 + q - k)
        elif delta == 0:
            idx[which] = np.where(k <= q, _bucket(q - k), 32)
        else:
            idx[which] = 32
    btab = np.zeros((8, 128, 3, 2, 256), np.float32)
    for h in range(8):
        ext = np.concatenate([rel_bias[:, h], np.array([NEG], np.float32)]).astype(np.float32)
        t = ext[idx]
        btab[h] = t.reshape(3, 2, 128, 256).transpose(2, 0, 1, 3)
    own_rows = np.concatenate([np.arange((2 * i + p) * 256, (2 * i + p + 1) * 256) for i in range(16)]).astype(np.int32)
    own_idx = np.ascontiguousarray(own_rows.reshape(32, 128).T)
    return gm.reshape(-1), btab.reshape(8, 128, 1536), own_idx, own_rows


def make_in_maps(inputs, cores, upto="H"):
    x = np.asarray(inputs["x"], np.float32)
    consts = make_consts()
    koh = np.zeros((32, S), np.float32)
    for j in range(32):
        koh[j, j * 256:(j + 1) * 256] = 1.0
    g = lambda n: np.ascontiguousarray(np.asarray(inputs[n], np.float32)[0])
    shared = {
        "rel31": np.ascontiguousarray(np.asarray(inputs["rel_bias"], np.float32)[31]),
        "w_ada": g("w_ada"), "b_ada": g("b_ada"), "norm_mix": g("norm_mix"), "w_in": g("w_in"),
        "w_gk_up": g("w_gk_up"), "b_gk": g("b_gk"), "gla_norm": g("gla_norm"), "w_pa": g("w_proj_moba"),
        "w_pb": g("w_proj_gla"), "w_out": g("w_out"), "norm_ffn": g("norm_ffn"), "w_router": g("w_router"),
        "b_router": g("b_router"),
        "bgu": np.ascontiguousarray(np.concatenate([g("b_gate"), g("b_up")], axis=1)),
        "b_down": g("b_down"), "norm_final": np.asarray(inputs["norm_final"], np.float32),
        "consts": consts, "koh": koh,
    }
    if upto >= "G":
        perm = lambda n: np.ascontiguousarray(g(n).reshape(32, 8, 128, D).transpose(0, 2, 1, 3)).reshape(32 * 128, 8 * D)
        shared["w_gate"] = perm("w_gate")
        shared["w_up"] = perm("w_up")
        shared["w_down"] = perm("w_down")
    maps = []
    rows = []
    rb = np.asarray(inputs["rel_bias"], np.float32)
    for core in cores:
        b, p = core // 2, core % 2
        gm, btab, own_idx, own_rows = make_core_tables(p, rb)
        m = dict(shared)
        m["xf"] = np.ascontiguousarray(x[b])
        m["xo"] = np.ascontiguousarray(x[b][own_rows])
        m["cvec"] = np.ascontiguousarray(np.asarray(inputs["c"], np.float32)[b])
        m["gm"] = gm
        m["btab"] = btab
        m["own_idx"] = own_idx
        maps.append(m)
        rows.append((b, own_rows))
    return maps, rows


_NC_CACHE = {}


def kernel(**inputs):
    if "H" not in _NC_CACHE:
        _NC_CACHE["H"] = Builder("H").build()
    nc = _NC_CACHE["H"]
    cores = list(range(8))
    maps, rows = make_in_maps(inputs, cores, "H")
    res = run_bass_kernel_spmd(nc, maps, core_ids=cores)
    outp = np.zeros((4, S, D), np.float32)
    for ci, (b, own_rows) in enumerate(rows):
        outp[b][own_rows] = res.results[ci]["out"]
    return outp
```
